# Optimizing a Trainium2 kernel written in Bass

```python
import jax
import jax.numpy as jnp
from jax import lax
import numpy as np

D_MODEL = 1024
BATCH = 4
SEQ = 4096
DEPTH = 1

HEAD_DIM = 128
N_HEADS_DN = 4
N_HEADS_MOBA = 4
D_DN = N_HEADS_DN * HEAD_DIM
D_MOBA = N_HEADS_MOBA * HEAD_DIM
D_MIX = D_DN + D_MOBA
CONV_K = 4
DN_CHUNK = 64
MOBA_BLOCK = 256
MOBA_TOPK = 3
MOBA_Q_CHUNK = 64
ROPE_THETA = 10000.0
N_GROUPS = 4
EXPERTS_PER_GROUP = 4
N_EXPERTS = N_GROUPS * EXPERTS_PER_GROUP
TOPK_IN_GROUP = 2
D_EXPERT = 256
LN_EPS = 1e-5
RMS_EPS = 1e-6
NEG_INF = -1e30
DEEPNORM_ALPHA = (2 * DEPTH) ** 0.25
DEEPNORM_BETA = (8 * DEPTH) ** -0.25

IN_PROJ_SIZES = (D_DN, D_DN, D_DN, D_DN, N_HEADS_DN, N_HEADS_DN, D_MOBA, D_MOBA, D_MOBA)
IN_PROJ_SPLITS = tuple(sum(IN_PROJ_SIZES[:i + 1]) for i in range(len(IN_PROJ_SIZES) - 1))
D_IN_PROJ = sum(IN_PROJ_SIZES)

kernel_name = 'hybrid_deltanet_moba_hmoe_deepnorm'


def causal_depthwise_conv(u, w):
    k_width, channels = w.shape
    return lax.conv_general_dilated(
        u, w[:, None, :].astype(u.dtype), window_strides=(1,), padding=[(k_width - 1, 0)],
        dimension_numbers=('NWC', 'WIO', 'NWC'), feature_group_count=channels)


def l2_normalize(t, eps=1e-6):
    return t * lax.rsqrt(jnp.sum(t * t, axis=-1, keepdims=True) + eps)


def layer_norm(t, g, b):
    tf = t.astype(jnp.float32)
    mu = jnp.mean(tf, axis=-1, keepdims=True)
    var = jnp.mean(jnp.square(tf - mu), axis=-1, keepdims=True)
    y = (tf - mu) * lax.rsqrt(var + LN_EPS) * g.astype(jnp.float32) + b.astype(jnp.float32)
    return y.astype(t.dtype)


def apply_rope(t, positions):
    half = HEAD_DIM // 2
    inv_freq = ROPE_THETA ** (-jnp.arange(half, dtype=jnp.float32) / half)
    ang = positions.astype(jnp.float32)[:, None] * inv_freq[None, :]
    cos, sin = jnp.cos(ang), jnp.sin(ang)
    tf = t.astype(jnp.float32)
    t1, t2 = tf[..., :half], tf[..., half:]
    return jnp.concatenate([t1 * cos - t2 * sin, t2 * cos + t1 * sin], axis=-1).astype(t.dtype)


def gated_deltanet(q, k, v, z, b, a, conv_w, a_log, dt_bias, norm_w):
    bsz, seq, _ = q.shape
    dtype = q.dtype
    f32 = jnp.float32
    qkv = jax.nn.silu(causal_depthwise_conv(jnp.concatenate([q, k, v], axis=-1), conv_w))
    q, k, v = jnp.split(qkv.astype(f32), 3, axis=-1)

    def heads(t):
        return t.reshape(bsz, seq, N_HEADS_DN, HEAD_DIM).transpose(0, 2, 1, 3)

    q = l2_normalize(heads(q)) * (HEAD_DIM ** -0.5)
    k = l2_normalize(heads(k))
    v = heads(v)
    beta = jax.nn.sigmoid(b.astype(f32)).transpose(0, 2, 1)
    g = (-jnp.exp(a_log.astype(f32)) *
         jax.nn.softplus(a.astype(f32) + dt_bias.astype(f32))).transpose(0, 2, 1)

    nc = seq // DN_CHUNK

    def chunks(t):
        return t.reshape(bsz, N_HEADS_DN, nc, DN_CHUNK, *t.shape[3:])

    q, k, v, beta, g = chunks(q), chunks(k), chunks(v), chunks(beta), chunks(g)
    g = jnp.cumsum(g, axis=-1)
    idx = jnp.arange(DN_CHUNK)
    incl = idx[:, None] >= idx[None, :]
    strict = idx[:, None] > idx[None, :]
    decay = jnp.exp(jnp.where(incl, g[..., :, None] - g[..., None, :], -jnp.inf))
    k_beta = k * beta[..., None]
    v_beta = v * beta[..., None]
    a_mat = jnp.where(strict, jnp.einsum('bhncd,bhnsd->bhncs', k_beta, k) * decay, 0.0)
    lhs = a_mat + jnp.eye(DN_CHUNK, dtype=f32)
    w_c = lax.linalg.triangular_solve(lhs, k_beta * jnp.exp(g)[..., None], left_side=True,
                                      lower=True, unit_diagonal=True)
    u_c = lax.linalg.triangular_solve(lhs, v_beta, left_side=True, lower=True, unit_diagonal=True)
    qk = jnp.einsum('bhncd,bhnsd->bhncs', q, k) * decay
    q_dec = q * jnp.exp(g)[..., None]
    k_dec = k * jnp.exp(g[..., -1:] - g)[..., None]
    g_last = jnp.exp(g[..., -1])

    def step(state, inp):
        qk_c, qd_c, wc, uc, kd_c, gl_c = inp
        v_new = uc - jnp.einsum('bhck,bhkv->bhcv', wc, state)
        o_c = jnp.einsum('bhck,bhkv->bhcv', qd_c, state) + jnp.einsum('bhcs,bhsv->bhcv', qk_c, v_new)
        state = state * gl_c[..., None, None] + jnp.einsum('bhck,bhcv->bhkv', kd_c, v_new)
        return state, o_c

    s0 = jnp.zeros((bsz, N_HEADS_DN, HEAD_DIM, HEAD_DIM), f32)
    xs = tuple(jnp.moveaxis(t, 2, 0) for t in (qk, q_dec, w_c, u_c, k_dec, g_last))
    _, o = lax.scan(step, s0, xs)
    o = jnp.moveaxis(o, 0, 2).reshape(bsz, N_HEADS_DN, seq, HEAD_DIM).transpose(0, 2, 1, 3)
    o = o * lax.rsqrt(jnp.mean(o * o, axis=-1, keepdims=True) + RMS_EPS) * norm_w.astype(f32)
    o = o * jax.nn.silu(z.astype(f32).reshape(bsz, seq, N_HEADS_DN, HEAD_DIM))
    return o.reshape(bsz, seq, D_DN).astype(dtype)


def moba_attention(q, k, v):
    bsz, seq, _ = q.shape
    dtype = q.dtype

    def heads(t):
        return t.reshape(bsz, seq, N_HEADS_MOBA, HEAD_DIM).transpose(0, 2, 1, 3)

    pos = jnp.arange(seq)
    q = apply_rope(heads(q), pos) * (HEAD_DIM ** -0.5)
    k = apply_rope(heads(k), pos)
    v = heads(v)
    nb = -(-seq // MOBA_BLOCK)
    pad = nb * MOBA_BLOCK - seq
    k = jnp.pad(k, ((0, 0), (0, 0), (0, pad), (0, 0)))
    v = jnp.pad(v, ((0, 0), (0, 0), (0, pad), (0, 0)))
    kb = k.reshape(bsz, N_HEADS_MOBA, nb, MOBA_BLOCK, HEAD_DIM)
    vb = v.reshape(bsz, N_HEADS_MOBA, nb, MOBA_BLOCK, HEAD_DIM)
    kmean = jnp.mean(kb.astype(jnp.float32), axis=3)
    topk = min(MOBA_TOPK, nb)
    gather_blocks = jax.vmap(jax.vmap(lambda blk, ix: blk[ix]))
    blk_ids = jnp.arange(nb)
    n_chunks = seq // MOBA_Q_CHUNK

    def chunk(c):
        start = c * MOBA_Q_CHUNK
        qc = lax.dynamic_slice_in_dim(q, start, MOBA_Q_CHUNK, axis=2)
        qpos = start + jnp.arange(MOBA_Q_CHUNK)
        own = start // MOBA_BLOCK
        gate = jnp.einsum('bhqd,bhnd->bhqn', qc.astype(jnp.float32), kmean)
        gate = jnp.where(blk_ids < own, gate, NEG_INF)
        _, sel = lax.top_k(gate, topk)
        valid = sel < own
        k_sel = gather_blocks(kb, sel)
        v_sel = gather_blocks(vb, sel)
        s_sel = jnp.einsum('bhqd,bhqnkd->bhqnk', qc, k_sel).astype(jnp.float32)
        s_sel = jnp.where(valid[..., None], s_sel, NEG_INF)
        s_sel = s_sel.reshape(bsz, N_HEADS_MOBA, MOBA_Q_CHUNK, topk * MOBA_BLOCK)
        k_own = lax.dynamic_index_in_dim(kb, own, axis=2, keepdims=False)
        v_own = lax.dynamic_index_in_dim(vb, own, axis=2, keepdims=False)
        s_own = jnp.einsum('bhqd,bhkd->bhqk', qc, k_own).astype(jnp.float32)
        kpos = own * MOBA_BLOCK + jnp.arange(MOBA_BLOCK)
        s_own = jnp.where(kpos[None, :] <= qpos[:, None], s_own, NEG_INF)
        p = jax.nn.softmax(jnp.concatenate([s_sel, s_own], axis=-1), axis=-1).astype(dtype)
        p_sel = p[..., :topk * MOBA_BLOCK].reshape(bsz, N_HEADS_MOBA, MOBA_Q_CHUNK, topk, MOBA_BLOCK)
        p_own = p[..., topk * MOBA_BLOCK:]
        return (jnp.einsum('bhqnk,bhqnkd->bhqd', p_sel, v_sel) +
                jnp.einsum('bhqk,bhkd->bhqd', p_own, v_own))

    out = lax.map(chunk, jnp.arange(n_chunks))
    out = out.transpose(1, 0, 3, 2, 4).reshape(bsz, seq, D_MOBA)
    return out.astype(dtype)


def hierarchical_moe(h, router_w1, router_b1, router_w2, router_b2, w_gate, w_up, w_down):
    bsz, seq, d = h.shape
    xt = h.reshape(-1, d)
    f32 = jnp.float32
    p_group = jax.nn.softmax(jnp.einsum('nd,dg->ng', xt, router_w1).astype(f32)
                             + router_b1.astype(f32), axis=-1)
    pg, gsel = lax.top_k(p_group, 1)
    logits2 = jnp.einsum('nd,gde->nge', xt, router_w2).astype(f32) + router_b2.astype(f32)
    logits2 = jnp.einsum('nge,ng->ne', logits2, jax.nn.one_hot(gsel[:, 0], N_GROUPS, dtype=f32))
    pe, esel = lax.top_k(jax.nn.softmax(logits2, axis=-1), TOPK_IN_GROUP)
    weights = pg * (pe / jnp.sum(pe, axis=-1, keepdims=True))
    expert_id = gsel * EXPERTS_PER_GROUP + esel
    gates = jnp.einsum('nk,nke->ne', weights,
                       jax.nn.one_hot(expert_id, N_EXPERTS, dtype=f32)).astype(xt.dtype)
    y = jnp.zeros_like(xt)
    for e in range(N_EXPERTS):
        he = jax.nn.silu(xt @ w_gate[e]) * (xt @ w_up[e])
        y = y + gates[:, e:e + 1] * (he @ w_down[e])
    return y.reshape(bsz, seq, d)


def setup_inputs(seed: int = 0) -> dict:
    key = jax.random.key(seed)
    ks = jax.random.split(key, 20)
    f32 = jnp.float32
    x = jax.random.normal(ks[0], (BATCH, SEQ, D_MODEL), f32)
    col_scale = np.ones((D_IN_PROJ,), np.float32)
    col_scale[2 * D_DN:3 * D_DN] = DEEPNORM_BETA
    col_scale[D_IN_PROJ - D_MOBA:] = DEEPNORM_BETA
    w_in = (jax.random.normal(ks[1], (DEPTH, D_MODEL, D_IN_PROJ), f32) * D_MODEL ** -0.5
            * jnp.asarray(col_scale))
    conv_w = jax.random.normal(ks[2], (DEPTH, CONV_K, 3 * D_DN), f32) * CONV_K ** -0.5
    a_log = jnp.log(jax.random.uniform(ks[3], (DEPTH, N_HEADS_DN), f32, minval=1.0, maxval=16.0))
    dt_bias = 1.0 + 0.1 * jax.random.normal(ks[4], (DEPTH, N_HEADS_DN), f32)
    dn_norm_w = 1.0 + 0.02 * jax.random.normal(ks[5], (DEPTH, HEAD_DIM), f32)
    w_out = jax.random.normal(ks[6], (DEPTH, D_MIX, D_MODEL), f32) * D_MIX ** -0.5 * DEEPNORM_BETA
    ln1_g = 1.0 + 0.02 * jax.random.normal(ks[7], (DEPTH, D_MODEL), f32)
    ln1_b = 0.02 * jax.random.normal(ks[8], (DEPTH, D_MODEL), f32)
    router_w1 = jax.random.normal(ks[9], (DEPTH, D_MODEL, N_GROUPS), f32) * D_MODEL ** -0.5
    router_b1 = 0.01 * jax.random.normal(ks[10], (DEPTH, N_GROUPS), f32)
    router_w2 = (jax.random.normal(ks[11], (DEPTH, N_GROUPS, D_MODEL, EXPERTS_PER_GROUP), f32)
                 * D_MODEL ** -0.5)
    router_b2 = 0.01 * jax.random.normal(ks[12], (DEPTH, N_GROUPS, EXPERTS_PER_GROUP), f32)
    expert_w_gate = (jax.random.normal(ks[13], (DEPTH, N_EXPERTS, D_MODEL, D_EXPERT), f32)
                     * D_MODEL ** -0.5)
    expert_w_up = (jax.random.normal(ks[14], (DEPTH, N_EXPERTS, D_MODEL, D_EXPERT), f32)
                   * D_MODEL ** -0.5 * DEEPNORM_BETA)
    expert_w_down = (jax.random.normal(ks[15], (DEPTH, N_EXPERTS, D_EXPERT, D_MODEL), f32)
                     * D_EXPERT ** -0.5 * DEEPNORM_BETA)
    ln2_g = 1.0 + 0.02 * jax.random.normal(ks[16], (DEPTH, D_MODEL), f32)
    ln2_b = 0.02 * jax.random.normal(ks[17], (DEPTH, D_MODEL), f32)
    return {'x': x, 'w_in': w_in, 'conv_w': conv_w, 'a_log': a_log, 'dt_bias': dt_bias,
            'dn_norm_w': dn_norm_w, 'w_out': w_out, 'ln1_g': ln1_g, 'ln1_b': ln1_b,
            'router_w1': router_w1, 'router_b1': router_b1, 'router_w2': router_w2,
            'router_b2': router_b2, 'expert_w_gate': expert_w_gate, 'expert_w_up': expert_w_up,
            'expert_w_down': expert_w_down, 'ln2_g': ln2_g, 'ln2_b': ln2_b}


def reference(x, w_in, conv_w, a_log, dt_bias, dn_norm_w, w_out, ln1_g, ln1_b,
              router_w1, router_b1, router_w2, router_b2, expert_w_gate, expert_w_up,
              expert_w_down, ln2_g, ln2_b):
    for l in range(DEPTH):
        proj = jnp.einsum('btd,dc->btc', x, w_in[l])
        q_dn, k_dn, v_dn, z_dn, b_dn, a_dn, q_mb, k_mb, v_mb = jnp.split(proj, IN_PROJ_SPLITS, axis=-1)
        y_dn = gated_deltanet(q_dn, k_dn, v_dn, z_dn, b_dn, a_dn, conv_w[l], a_log[l],
                              dt_bias[l], dn_norm_w[l])
        y_mb = moba_attention(q_mb, k_mb, v_mb)
        mix = jnp.einsum('btc,cd->btd', jnp.concatenate([y_dn, y_mb], axis=-1), w_out[l])
        h = layer_norm(DEEPNORM_ALPHA * x + mix, ln1_g[l], ln1_b[l])
        ffn = hierarchical_moe(h, router_w1[l], router_b1[l], router_w2[l], router_b2[l],
                               expert_w_gate[l], expert_w_up[l], expert_w_down[l])
        x = layer_norm(DEEPNORM_ALPHA * h + ffn, ln2_g[l], ln2_b[l])
    return x
```

```python
import contextlib
import numpy as np
import concourse.bass as bass
import concourse.mybir as mybir
from concourse.bass_utils import run_bass_kernel_spmd

AF = mybir.ActivationFunctionType
ALU = mybir.AluOpType
AX = mybir.AxisListType
F32 = mybir.dt.float32
BF16 = mybir.dt.bfloat16

ENGS = ['pe', 'act', 'dve', 'pool', 'sp']

D = 1024
T = 4096
TOWN = 2048
NT_OWN = TOWN // 128
ALPHA = 2.0 ** 0.25
LN_EPS = 1e-5
NEXP = 16
DEXP = 256
BIG = 30000.0


class _Op:
    __slots__ = ('eng', 'idx', 'fn', 'deps', 'ms', 'cnt', 'ndma', 'semkey', 'dmaval')

    def __init__(self, eng, idx, fn, ndma, semkey):
        self.eng = eng
        self.idx = idx
        self.fn = fn
        self.deps = []
        self.ms = False
        self.cnt = 0
        self.ndma = ndma
        self.semkey = semkey
        self.dmaval = 0


class SemPool:
    def __init__(self, nc, es):
        self.nc = nc
        self.es = es
        self.engsem = {e: es.enter_context(nc.semaphore('s_' + e)) for e in ENGS}
        self.engbase = {e: 0 for e in ENGS}
        self.dmasem = {}
        self.dma_tot = {}

    def dsem(self, key):
        if key not in self.dmasem:
            self.dmasem[key] = self.es.enter_context(self.nc.semaphore('d%d' % len(self.dmasem)))
            self.dma_tot[key] = 0
        return self.dmasem[key]


ATTACH_WAIT = True


class Sched:
    def __init__(self, nc, pool):
        self.nc = nc
        self.pool = pool
        self.ops = {e: [] for e in ENGS}
        self.lastw = {}
        self.readers = {}
        self.used_dma = set()

    LIMIT = [10 ** 9]
    COUNT = [0]

    def add(self, eng, fn, reads=(), writes=(), ndma=0, semkey=None):
        Sched.COUNT[0] += 1
        if Sched.COUNT[0] > Sched.LIMIT[0] and not (semkey == 'yo'):
            return None
        lst = self.ops[eng]
        op = _Op(eng, len(lst), fn, ndma, semkey)
        if ndma:
            assert semkey is not None
            self.pool.dsem(semkey)
            self.pool.dma_tot[semkey] += 16 * ndma
            op.dmaval = self.pool.dma_tot[semkey]
            self.used_dma.add(semkey)
        deps = {}
        for r in reads:
            w = self.lastw.get(r)
            if w is not None:
                deps[id(w)] = (w, True)
            if isinstance(r, tuple) and r[0] == 'bank':
                for rd in self.readers.get(r, ()):
                    if rd.eng != eng and id(rd) not in deps:
                        deps[id(rd)] = (rd, True)
        for r in writes:
            w = self.lastw.get(r)
            if w is not None and id(w) not in deps:
                deps[id(w)] = (w, False)
            for rd in self.readers.get(r, ()):
                if id(rd) not in deps:
                    deps[id(rd)] = (rd, False)
        for d, raw in deps.values():
            if d is op:
                continue
            if d.eng == eng and not d.ndma and eng == 'pe':
                continue
            op.deps.append(d)
            if not d.ndma:
                d.ms = True
        for r in reads:
            self.readers.setdefault(r, []).append(op)
        for r in writes:
            self.lastw[r] = op
            self.readers[r] = []
        lst.append(op)
        return op

    def emit(self):
        nc = self.nc
        pool = self.pool
        engsem, dmasem = pool.engsem, pool.dmasem
        for e in ENGS:
            cands = [o for o in self.ops[e] if not o.ndma]
            if e != 'sp' and cands:
                cands[-1].ms = True
        for e in ENGS:
            c = pool.engbase[e]
            for op in self.ops[e]:
                if op.ms and not op.ndma:
                    c += 1
                op.cnt = c
            pool.engbase[e] = c
        with nc.Block() as block:
            def run(ename, eng):
                waited = {}
                for op in self.ops[ename]:
                    need = {}
                    for d in op.deps:
                        if d.ndma:
                            s, v = dmasem[d.semkey], d.dmaval
                        else:
                            s, v = engsem[d.eng], d.cnt
                        key = id(s)
                        if key not in need or need[key][1] < v:
                            need[key] = (s, v)
                    pend = [(key, s, v) for key, (s, v) in need.items() if waited.get(key, 0) < v]
                    for key, s, v in pend:
                        waited[key] = v
                    attach = None
                    if ATTACH_WAIT and len(pend) >= 1 and not op.ndma:
                        attach = pend.pop()
                    for key, s, v in pend:
                        eng.wait_ge(s, v)
                    ins = op.fn(eng)
                    if attach is not None:
                        ins._wait_ge(attach[1], attach[2])
                    if op.ndma:
                        if not isinstance(ins, (list, tuple)):
                            ins = [ins]
                        assert len(ins) == op.ndma, (len(ins), op.ndma)
                        for i in ins:
                            i.then_inc(dmasem[op.semkey], 16)
                    elif op.ms:
                        ins.then_inc(engsem[ename], 1)
                if ename == 'sp':
                    for k in self.used_dma:
                        eng.wait_ge(dmasem[k], pool.dma_tot[k])
                    for e2 in ENGS:
                        if e2 != 'sp' and pool.engbase[e2]:
                            eng.wait_ge(engsem[e2], pool.engbase[e2])

            @block.tensor
            def _(eng):
                run('pe', eng)

            @block.scalar
            def _(eng):
                run('act', eng)

            @block.vector
            def _(eng):
                run('dve', eng)

            @block.gpsimd
            def _(eng):
                run('pool', eng)

            @block.sync
            def _(eng):
                run('sp', eng)


class Ctx:
    pass


def _psum_banks(nc, es, n=8):
    return [es.enter_context(nc.psum_tensor('bank%d' % i, [128, 512], F32)) for i in range(n)]


def build_p2a(nc, S, C, es):
    sb = lambda name, shape, dt: es.enter_context(nc.sbuf_tensor(name, shape, dt))
    wout = sb('wout', [128, 8, D], BF16)
    g1 = sb('g1', [128, D], F32)
    b1 = sb('b1', [128, D], F32)
    wr = sb('wr', [128, 8, 20], F32)
    rb = sb('rb', [128, 20], F32)
    xt = [sb('xt%d' % i, [128, D], F32) for i in range(3)]
    rr = [sb('rr%d' % i, [128, D], F32) for i in range(3)]
    hb = [sb('hb%d' % i, [128, D], BF16) for i in range(3)]
    h32T = [sb('h32T%d' % i, [128, 8, 128], F32) for i in range(3)]
    st = [sb('st%d' % i, [128, 2, 6], F32) for i in range(3)]
    mv = [sb('mv%d' % i, [128, 2], F32) for i in range(3)]
    sm = [sb('sm%d' % i, [128, 64], F32) for i in range(3)]
    banks = C.banks

    for c in range(8):
        S.add('pool', lambda e, c=c: e.dma_start(out=wout[:, c, :], in_=C.d_wout[:, c, :]),
              writes=[('wout', c)], ndma=1, semkey=('wout', c))
    S.add('sp', lambda e: e.dma_start(out=g1[:], in_=C.d_ln[0]), writes=['g1'], ndma=1, semkey='c1')
    S.add('sp', lambda e: e.dma_start(out=b1[:], in_=C.d_ln[1]), writes=['b1'], ndma=1, semkey='c2')
    S.add('sp', lambda e: e.dma_start(out=wr[:], in_=C.d_wr), writes=['wr'], ndma=1, semkey='c3')
    S.add('sp', lambda e: e.dma_start(out=rb[:], in_=C.d_rb), writes=['rb'], ndma=1, semkey='c4')

    def tile(t):
        p = t % 3
        X, R, HB, HT, ST, MV, SM = xt[p], rr[p], hb[p], h32T[p], st[p], mv[p], sm[p]
        rX, rR, rHB, rHT, rST, rMV, rSM = ('xt', p), ('rr', p), ('hb', p), ('h32T', p), ('st', p), ('mv', p), ('sm', p)
        tok = slice(t * 128, (t + 1) * 128)
        S.add('sp', lambda e, X=X, t=t: e.dma_start(out=X[:], in_=C.d_xown[t * 128:(t + 1) * 128, :]),
              writes=[rX], ndma=1, semkey=('xt', p))
        yield
        for half in range(2):
            bk = banks[half]
            for c in range(8):
                S.add('pe', lambda e, bk=bk, c=c, half=half, tok=tok: e.matmul(
                    bk[:], lhsT=C.yT[:, c, tok], rhs=wout[:, c, half * 512:(half + 1) * 512],
                    start=(c == 0), stop=(c == 7)),
                    reads=[('wout', c), 'yT'], writes=[('bank', half)])
            S.add('dve', lambda e, bk=bk, half=half, X=X, R=R: e.scalar_tensor_tensor(
                out=R[:, half * 512:(half + 1) * 512], in0=X[:, half * 512:(half + 1) * 512], scalar=ALPHA,
                in1=bk[:], op0=ALU.mult, op1=ALU.add),
                reads=[rX, ('bank', half)], writes=[(rR, half)])
            yield
            S.add('dve', lambda e, half=half, R=R, ST=ST: e.bn_stats(out=ST[:, half, :], in_=R[:, half * 512:(half + 1) * 512]),
                  reads=[(rR, half)], writes=[(rST, half)])
            yield
        yield from _ln_tail(S, R, rR, ST, rST, MV, rMV, SM, rSM, g1, 'g1', b1, 'b1')
        S.add('act', lambda e, R=R, t=t: e.activation(out=C.ysb[:, t, :], in_=R[:], func=AF.Copy, scale=ALPHA),
              reads=[(rR, 0), (rR, 1)], writes=[('ysb', t)])
        yield
        S.add('pool', lambda e, R=R, HB=HB: e.tensor_copy(out=HB[:], in_=R[:]),
              reads=[(rR, 0), (rR, 1)], writes=[rHB])
        yield
        pt = C.bank_bf[2]
        for c in range(8):
            S.add('pe', lambda e, c=c, HB=HB, pt=pt: e.transpose(pt[:, c * 128:(c + 1) * 128], HB[:, c * 128:(c + 1) * 128], C.ident_bf[:]),
                  reads=[rHB, 'ident', 'ident_f0'], writes=[('bank', 2)])
        S.add('act', lambda e, pt=pt, tok=tok: e.activation(out=C.hT[:, :, tok], in_=pt[:, 0:1024].rearrange("p (c t) -> p c t", c=8), func=AF.Copy),
              reads=[('bank', 2)], writes=[('hT', t)])
        yield
        for q4 in range(2):
            bkq = banks[3 + q4]
            for c4 in range(4):
                c = q4 * 4 + c4
                S.add('pe', lambda e, c=c, c4=c4, R=R, bkq=bkq: e.transpose(bkq[:, c4 * 128:(c4 + 1) * 128], R[:, c * 128:(c + 1) * 128], C.ident_f[:]),
                      reads=[(rR, 0), (rR, 1), 'ident', 'ident_f0'], writes=[('bank', 3 + q4)])
            S.add('dve', lambda e, bkq=bkq, q4=q4, HT=HT: e.tensor_copy(out=HT[:, q4 * 4:(q4 + 1) * 4, :], in_=bkq[:].rearrange("p (c t) -> p c t", c=4)),
                  reads=[('bank', 3 + q4)], writes=[(rHT, q4)])
            yield
        lg = banks[5]
        for c in range(8):
            S.add('pe', lambda e, c=c, HT=HT, lg=lg: e.matmul(lg[:, 0:20], lhsT=HT[:, c, :], rhs=wr[:, c, :], start=(c == 0), stop=(c == 7)),
                  reads=[(rHT, 0), (rHT, 1), 'wr'], writes=[('bank', 5)])
        yield from _router(S, C, lg, SM, rSM, rb, t)

    _rolling([(lambda t=t: tile(t)) for t in range(NT_OWN)], 3, 10)


def _ln_tail(S, R, rR, ST, rST, MV, rMV, SM, rSM, g, rg, b, rb_):
    both = [(rR, 0), (rR, 1)]
    S.add('dve', lambda e: e.bn_aggr(out=MV[:], in_=ST[:].rearrange("p a b -> p (a b)")),
          reads=[(rST, 0), (rST, 1)], writes=[rMV])
    yield
    S.add('act', lambda e: e.activation(out=SM[:, 0:1], in_=MV[:, 1:2], func=AF.Ln, bias=C_EPS_AP[0][:, 0:1], scale=1.0),
          reads=[rMV, 'eps'], writes=[(rSM, 'a')])
    yield
    S.add('act', lambda e: e.activation(out=SM[:, 1:2], in_=SM[:, 0:1], func=AF.Exp, scale=-0.5), reads=[(rSM, 'a')], writes=[(rSM, 'b')])
    yield
    S.add('dve', lambda e: e.tensor_scalar(out=R[:], in0=R[:], scalar1=MV[:, 0:1], scalar2=SM[:, 1:2], op0=ALU.subtract, op1=ALU.mult),
          reads=both + [rMV, (rSM, 'b')], writes=both)
    yield
    S.add('pool', lambda e: e.tensor_tensor(out=R[:], in0=R[:], in1=g[:], op=ALU.mult), reads=both + [rg], writes=both)
    yield
    S.add('pool', lambda e: e.tensor_tensor(out=R[:], in0=R[:], in1=b[:], op=ALU.add), reads=both + [rb_], writes=both)
    yield


C_EPS_AP = [None]


def _router(S, C, lg, SM, rSM, rb, t):
    L = SM[:, 8:28]
    r = lambda k: (rSM, k)
    S.add('dve', lambda e: e.tensor_tensor(out=L, in0=lg[:, 0:20], in1=rb[:], op=ALU.add),
          reads=[('bank', 5), 'rb'], writes=[r('L')])
    yield
    S.add('dve', lambda e: e.tensor_reduce(out=SM[:, 3:4], in_=SM[:, 8:12], axis=AX.X, op=ALU.max, negate=True), reads=[r('L')], writes=[r('nm1')])
    yield
    S.add('act', lambda e: e.activation(out=SM[:, 28:32], in_=SM[:, 8:12], func=AF.Exp, bias=SM[:, 3:4], scale=1.0, accum_out=SM[:, 4:5]),
          reads=[r('L'), r('nm1')], writes=[r('e1'), r('s1')])
    yield
    S.add('dve', lambda e: e.tensor_scalar(out=SM[:, 32:36], in0=SM[:, 8:12], scalar1=SM[:, 3:4], scalar2=0.0, op0=ALU.add, op1=ALU.is_ge), reads=[r('L'), r('nm1')], writes=[r('oh')])
    yield
    S.add('dve', lambda e: e.tensor_scalar(out=SM[:, 32:36], in0=SM[:, 32:36], scalar1=-1.0, scalar2=1e30, op0=ALU.add, op1=ALU.mult), reads=[r('oh')], writes=[r('oh')])
    yield
    S.add('dve', lambda e: e.tensor_tensor(out=SM[:, 12:28].rearrange("p (g x) -> p g x", g=4), in0=SM[:, 12:28].rearrange("p (g x) -> p g x", g=4),
                                           in1=SM[:, 32:36].unsqueeze(2).to_broadcast([128, 4, 4]), op=ALU.add),
          reads=[r('L'), r('oh')], writes=[r('L')])
    yield
    S.add('dve', lambda e: e.tensor_reduce(out=SM[:, 6:7], in_=SM[:, 12:28], axis=AX.X, op=ALU.max, negate=True), reads=[r('L')], writes=[r('nm2')])
    yield
    S.add('act', lambda e: e.activation(out=SM[:, 36:52], in_=SM[:, 12:28], func=AF.Exp, bias=SM[:, 6:7], scale=1.0),
          reads=[r('L'), r('nm2')], writes=[r('p2')])
    yield
    S.add('dve', lambda e: e.max(out=SM[:, 52:60], in_=SM[:, 36:52]), reads=[r('p2')], writes=[r('top')])
    yield
    S.add('dve', lambda e: e.scalar_tensor_tensor(out=SM[:, 7:8], in0=SM[:, 52:53], scalar=SM[:, 53:54], in1=SM[:, 4:5], op0=ALU.add, op1=ALU.mult),
          reads=[r('top'), r('s1')], writes=[r('den')])
    yield
    S.add('dve', lambda e: e.reciprocal(out=SM[:, 60:61], in_=SM[:, 7:8]), reads=[r('den')], writes=[r('wsc')])
    yield
    S.add('dve', lambda e: e.tensor_scalar(out=SM[:, 12:28], in0=SM[:, 36:52], scalar1=SM[:, 53:54], scalar2=None, op0=ALU.is_ge), reads=[r('p2'), r('top'), r('L')], writes=[r('L')])
    yield
    S.add('dve', lambda e: e.scalar_tensor_tensor(out=SM[:, 36:52], in0=SM[:, 36:52], scalar=SM[:, 60:61], in1=SM[:, 12:28], op0=ALU.mult, op1=ALU.mult),
          reads=[r('p2'), r('wsc'), r('L')], writes=[r('p2')])
    yield
    gt = C.banks[6]
    S.add('pe', lambda e: e.transpose(gt[0:16, 0:128], SM[:, 36:52], C.ident_f[:]), reads=[r('p2'), 'ident', 'ident_f0'], writes=[('bank', 6)])
    S.add('act', lambda e: e.activation(out=C.gatesT[:, t * 128:(t + 1) * 128], in_=gt[0:16, 0:128], func=AF.Copy),
          reads=[('bank', 6)], writes=[('gatesT', t)])
    yield


def build_p2b(nc, S, C, es):
    sb = lambda name, shape, dt: es.enter_context(nc.sbuf_tensor(name, shape, dt))
    wg = [sb('wg%d' % i, [128, 2, 8, DEXP], BF16) for i in range(2)]
    wu = [sb('wu%d' % i, [128, 2, 8, DEXP], BF16) for i in range(2)]
    wd = [sb('wd%d' % i, [128, 2, 2, D], BF16) for i in range(2)]
    g2 = sb('g2', [128, D], F32)
    b2 = sb('b2', [128, D], F32)
    sg = [sb('sg%d' % i, [128, 512], F32) for i in range(2)]
    t1 = [sb('t1%d' % i, [128, 512], F32) for i in range(2)]
    he = [sb('he%d' % i, [128, 256], BF16) for i in range(4)]
    st = [sb('st2%d' % i, [128, 2, 6], F32) for i in range(2)]
    mv = [sb('mv2%d' % i, [128, 2], F32) for i in range(2)]
    sm = [sb('sm2%d' % i, [128, 8], F32) for i in range(2)]
    banks = C.banks
    S.add('sp', lambda e: e.dma_start(out=g2[:], in_=C.d_ln[2]), writes=['g2'], ndma=1, semkey='c1')
    S.add('sp', lambda e: e.dma_start(out=b2[:], in_=C.d_ln[3]), writes=['b2'], ndma=1, semkey='c2')
    def ln2(t):
            p = t % 2
            ST, MV, SM = st[p], mv[p], sm[p]
            R = C.ysb[:, t, :]

            rR = ('ysbx', t)
            for half in range(2):
                S.add('dve', lambda e, half=half, R=R, ST=ST: e.bn_stats(out=ST[:, half, :], in_=R[:, half * 512:(half + 1) * 512]),
                      reads=[('ysb', t)], writes=[(('st2', p), half)])
            both = [('ysb', t)]
            S.add('dve', lambda e, MV=MV, ST=ST: e.bn_aggr(out=MV[:], in_=ST[:].rearrange("p a b -> p (a b)")),
                  reads=[(('st2', p), 0), (('st2', p), 1)], writes=[('mv2', p)])
            S.add('act', lambda e, SM=SM, MV=MV: e.activation(out=SM[:, 0:1], in_=MV[:, 1:2], func=AF.Ln, bias=C_EPS_AP[0][:, 0:1], scale=1.0),
                  reads=[('mv2', p), 'eps'], writes=[('sm2', p, 'a')])
            S.add('act', lambda e, SM=SM: e.activation(out=SM[:, 1:2], in_=SM[:, 0:1], func=AF.Exp, scale=-0.5), reads=[('sm2', p, 'a')], writes=[('sm2', p, 'b')])
            S.add('dve', lambda e, R=R, MV=MV, SM=SM: e.tensor_scalar(out=R, in0=R, scalar1=MV[:, 0:1], scalar2=SM[:, 1:2], op0=ALU.subtract, op1=ALU.mult),
                  reads=both + [('mv2', p), ('sm2', p, 'b')], writes=both)
            S.add('pool', lambda e, R=R: e.tensor_tensor(out=R, in0=R, in1=g2[:], op=ALU.mult), reads=both + ['g2'], writes=both)
            S.add('pool', lambda e, R=R: e.tensor_tensor(out=R, in0=R, in1=b2[:], op=ALU.add), reads=both + ['b2'], writes=both)
            S.add('sp', lambda e, R=R, t=t: e.dma_start(out=C.d_out[t * 128:(t + 1) * 128, :], in_=R), reads=both, writes=[('out', t)],
                  ndma=1, semkey=('out', t % 4))

    k = 0
    for ep in range(NEXP // 2):
        p = ep % 2
        S.add('pool', lambda e, p=p, ep=ep: e.dma_start(out=wg[p][:], in_=C.d_wg[2 * ep:2 * ep + 2].rearrange("e p c n -> p e c n"), max_dma_last_dim=8192),
              writes=[('wexp', p, 'g')], ndma=1, semkey=('wexp', p, 'g'))
        S.add('pool', lambda e, p=p, ep=ep: e.dma_start(out=wu[p][:], in_=C.d_wu[2 * ep:2 * ep + 2].rearrange("e p c n -> p e c n"), max_dma_last_dim=8192),
              writes=[('wexp', p, 'u')], ndma=1, semkey=('wexp', p, 'u'))
        S.add('pool', lambda e, p=p, ep=ep: e.dma_start(out=wd[p][:], in_=C.d_wd[2 * ep:2 * ep + 2].rearrange("e p c n -> p e c n"), max_dma_last_dim=8192),
              writes=[('wexp', p, 'd')], ndma=1, semkey=('wexp', p, 'd'))
        for tg in range(TOWN // 256):
            tk = slice(tg * 256, (tg + 1) * 256)
            for el in range(2):
                eg = 2 * ep + el
                gbk = 6 + (k % 2)
                for jc in range(2):
                    for c in range(8):
                        S.add('pe', lambda e, p=p, el=el, c=c, jc=jc, tk=tk: e.matmul(
                            banks[4][:, jc * 256:(jc + 1) * 256], lhsT=wg[p][:, el, c, jc * 128:(jc + 1) * 128], rhs=C.hT[:, c, tk], start=(c == 0), stop=(c == 7)),
                            reads=[('wexp', p, 'g'), ('hT', 2 * tg), ('hT', 2 * tg + 1)], writes=[('bank', 4)])
                for jc in range(2):
                    for c in range(8):
                        S.add('pe', lambda e, p=p, el=el, c=c, jc=jc, tk=tk: e.matmul(
                            banks[5][:, jc * 256:(jc + 1) * 256], lhsT=wu[p][:, el, c, jc * 128:(jc + 1) * 128], rhs=C.hT[:, c, tk], start=(c == 0), stop=(c == 7)),
                            reads=[('wexp', p, 'u'), ('hT', 2 * tg), ('hT', 2 * tg + 1)], writes=[('bank', 5)])
                S.add('pe', lambda e, eg=eg, tk=tk, gbk=gbk: e.matmul(
                    banks[gbk][:, 0:256], lhsT=C.esel[:, eg, :], rhs=C.gatesT[:, tk], start=True, stop=True),
                    reads=['esel', ('gatesT', 2 * tg), ('gatesT', 2 * tg + 1)], writes=[('bank', gbk)])
                SG, T1 = sg[k % 2], t1[k % 2]
                S.add('act', lambda e, SG=SG: e.activation(out=SG[:], in_=banks[4][:], func=AF.Silu),
                      reads=[('bank', 4)], writes=[('sg', k % 2)])
                S.add('dve', lambda e, SG=SG, T1=T1: e.tensor_tensor(out=T1[:], in0=banks[5][:], in1=SG[:], op=ALU.mult),
                      reads=[('bank', 5), ('sg', k % 2)], writes=[('t1', k % 2)])
                for jc in range(2):
                    hi = (2 * k + jc) % 4
                    HE = he[hi]
                    S.add('dve', lambda e, T1=T1, HE=HE, gbk=gbk, jc=jc: e.tensor_tensor(out=HE[:], in0=banks[gbk][:, 0:256], in1=T1[:, jc * 256:(jc + 1) * 256], op=ALU.mult),
                          reads=[('bank', gbk), ('t1', k % 2)], writes=[('he', hi)])
                for jc in range(2):
                    hi = (2 * k + jc) % 4
                    HE = he[hi]
                    first = (el == 0 and jc == 0)
                    last = (el == 1 and jc == 1)
                    for tl in range(2):
                        for half in range(2):
                            yb = tl * 2 + half
                            S.add('pe', lambda e, yb=yb, HE=HE, tl=tl, half=half, p=p, el=el, jc=jc, first=first, last=last: e.matmul(
                                banks[yb][:], lhsT=HE[:, tl * 128:(tl + 1) * 128], rhs=wd[p][:, el, jc, half * 512:(half + 1) * 512],
                                start=first, stop=last),
                                reads=[('he', hi), ('wexp', p, 'd')], writes=[('bank', yb)])
                k += 1
            for tl in range(2):
                tt = 2 * tg + tl
                for half in range(2):
                    yb = tl * 2 + half
                    S.add('dve', lambda e, yb=yb, tt=tt, half=half: e.tensor_tensor(
                        out=C.ysb[:, tt, half * 512:(half + 1) * 512], in0=banks[yb][:], in1=C.ysb[:, tt, half * 512:(half + 1) * 512], op=ALU.add),
                        reads=[('bank', yb), ('ysb', tt)], writes=[('ysb', tt)])
                if ep == NEXP // 2 - 1:
                    ln2(tt)
def consts_setup(nc, S, C, es):
    sb = lambda name, shape, dt: es.enter_context(nc.sbuf_tensor(name, shape, dt))
    C.ident_f = sb('ident_f', [128, 128], F32)
    C.ident_bf = sb('ident_bf', [128, 128], BF16)
    C.esel = sb('esel', [16, 16, 128], BF16)
    C.eps = sb('epsc', [128, 4], F32)
    C_EPS_AP[0] = C.eps
    S.add('sp', lambda e: e.dma_start(out=C.ident_f[:], in_=C.d_ident), writes=['ident_f0'], ndma=1, semkey='k1')
    S.add('pool', lambda e: e.dma_start(out=C.esel[:], in_=C.d_esel), writes=['esel'], ndma=1, semkey='k2')
    S.add('dve', lambda e: e.tensor_copy(out=C.ident_bf[:], in_=C.ident_f[:]), reads=['ident_f0'], writes=['ident'])
    S.add('dve', lambda e: e.memset(C.eps[:, 0:1], LN_EPS), writes=['eps0'])
    S.add('dve', lambda e: e.memset(C.eps[:, 1:2], 1e-6), writes=['eps1'])
    S.add('dve', lambda e: e.memset(C.eps[:, 2:3], float(np.log(128.0 ** -0.5))), writes=['eps2'])
    S.add('dve', lambda e: e.memset(C.eps[:, 3:4], 1.0), reads=['eps0', 'eps1', 'eps2'], writes=['eps'])


def host_consts():
    ident = np.eye(128, dtype=np.float32)
    esel = np.zeros((16, 16, 128), np.float32)
    for e in range(16):
        esel[e, e, :] = 1.0
    return {'c_ident': ident, 'c_esel': esel}


def declare_common_dram(nc, C):
    di = lambda name, shape: nc.dram_tensor(name, shape, F32, kind="ExternalInput").ap()
    C.d_ident = di('c_ident', [128, 128])
    C.d_esel = di('c_esel', [16, 16, 128])
    C.d_xown = di('x_own', [TOWN, D])
    C.d_wout = di('w_out', [128, 8, D])
    C.d_ln = [di('ln%d' % i, [128, D]) for i in range(4)]
    C.d_wr = di('w_r', [128, 8, 20])
    C.d_rb = di('b_r', [128, 20])
    C.d_wg = di('w_g', [NEXP, 128, 8, DEXP])
    C.d_wu = di('w_u', [NEXP, 128, 8, DEXP])
    C.d_wd = di('w_d', [NEXP, 128, 2, D])
    C.d_out = nc.dram_tensor('out', [TOWN, D], F32, kind="ExternalOutput").ap()


def host_common(inp, b, s):
    m = {}
    m.update(host_consts())
    m['x_own'] = np.ascontiguousarray(inp['x'][b, s * TOWN:(s + 1) * TOWN, :])
    m['w_out'] = np.ascontiguousarray(inp['w_out'][0].reshape(8, 128, D).transpose(1, 0, 2))
    for i, k in enumerate(['ln1_g', 'ln1_b', 'ln2_g', 'ln2_b']):
        m['ln%d' % i] = np.ascontiguousarray(np.broadcast_to(inp[k][0][None, :], (128, D)))
    wr = np.concatenate([inp['router_w1'][0]] + [inp['router_w2'][0, g] for g in range(4)], axis=1)
    m['w_r'] = np.ascontiguousarray(wr.reshape(8, 128, 20).transpose(1, 0, 2))
    br = np.concatenate([inp['router_b1'][0], inp['router_b2'][0].reshape(-1)])
    m['b_r'] = np.ascontiguousarray(np.broadcast_to(br[None, :], (128, 20)))
    m['w_g'] = np.ascontiguousarray(inp['expert_w_gate'][0].reshape(NEXP, 8, 128, DEXP).transpose(0, 2, 1, 3))
    m['w_u'] = np.ascontiguousarray(inp['expert_w_up'][0].reshape(NEXP, 8, 128, DEXP).transpose(0, 2, 1, 3))
    m['w_d'] = np.ascontiguousarray(inp['expert_w_down'][0].reshape(NEXP, 2, 128, D).transpose(0, 2, 1, 3))
    return m


def build_p2_test():
    nc = bass.Bass("TRN2", target_bir_lowering=False)
    C = Ctx()
    declare_common_dram(nc, C)
    d_yT = nc.dram_tensor('yT_dbg', [128, 8, TOWN], F32, kind="ExternalInput").ap()
    with contextlib.ExitStack() as es:
        sb = lambda name, shape, dt: es.enter_context(nc.sbuf_tensor(name, shape, dt))
        C.banks = _psum_banks(nc, es)
        C.bank_bf = [b[:].bitcast(BF16) for b in C.banks]
        C.yT = sb('yT', [128, 8, TOWN], BF16)
        P = SemPool(nc, es)
        S0 = Sched(nc, P)
        consts_setup(nc, S0, C, es)
        S0.add('pool', lambda e: e.dma_start(out=C.yT[:], in_=d_yT, max_dma_last_dim=8192), writes=['yT'], ndma=1, semkey='yT')
        S0.emit()
        nc.all_engine_barrier()
        with contextlib.ExitStack() as es2:
            sb2 = lambda name, shape, dt: es2.enter_context(nc.sbuf_tensor(name, shape, dt))
            C.ysb = sb2('ysb', [128, NT_OWN, D], F32)
            C.hT = sb2('hT', [128, 8, TOWN], BF16)
            C.gatesT = sb2('gatesT', [16, TOWN], BF16)
            with contextlib.ExitStack() as es3:
                S1 = Sched(nc, P)
                build_p2a(nc, S1, C, es3)
                S1.emit()
            nc.all_engine_barrier()
            with contextlib.ExitStack() as es4:
                S2 = Sched(nc, P)
                build_p2b(nc, S2, C, es4)
                S2.emit()
    return nc


NH = 4
HD = 128
SCALE = HD ** -0.5
NBLK = 16


def declare_p1_dram(nc, C):
    di = lambda name, shape: nc.dram_tensor(name, shape, F32, kind="ExternalInput").ap()
    C.d_xT = di('xT', [128, 8, T])
    C.d_wmb = di('w_mb', [NH, 128, 8, 384])
    C.d_cos = di('c_cos', [128, T])
    C.d_sin = di('c_sin', [128, T])
    C.d_prot = di('c_prot', [128, 128])
    C.d_cbias = di('c_cbias', [128, NBLK, NBLK])
    C.d_caus = di('c_caus', [2, 128, 256])
    C.d_sel = di('sel', [128, 2])
    C.d_xTo = di('xT_own', [128, 8, TOWN])
    C.d_coso = di('c_cos_own', [128, TOWN])
    C.d_sino = di('c_sin_own', [128, TOWN])
    C.d_cbo = di('c_cbias_own', [128, NT_OWN, NBLK])
    C.d_vmo = di('c_vmask_own', [128, 2, NT_OWN, NBLK])
    C.d_sela = di('sel_a', [128, 1])


def host_p1_consts():
    m = {}
    half = HD // 2
    inv_freq = (10000.0 ** (-np.arange(half, dtype=np.float32) / half)).astype(np.float32)
    ang = np.arange(T, dtype=np.float32)[None, :] * inv_freq[:, None]
    cos, sin = np.cos(ang).astype(np.float32), np.sin(ang).astype(np.float32)
    m['c_cos'] = np.ascontiguousarray(np.concatenate([cos, cos], 0))
    m['c_sin'] = np.ascontiguousarray(np.concatenate([-sin, sin], 0))
    prot = np.zeros((128, 128), np.float32)
    for mm in range(128):
        prot[(mm + 64) % 128, mm] = 1.0
    m['c_prot'] = prot
    cb = np.zeros((NBLK, NBLK), np.float32)
    for own in range(NBLK):
        cb[own, own:] = -1e30
    m['c_cbias'] = np.ascontiguousarray(np.broadcast_to(cb[None], (128, NBLK, NBLK)))
    caus = np.zeros((2, 128, 256), np.float32)
    for kh in range(2):
        kpos = kh * 128 + np.arange(128)[:, None]
        qpos = np.arange(256)[None, :]
        caus[kh] = np.where(kpos <= qpos, 0.0, -BIG)
    m['c_caus'] = caus
    return m


def host_p1(inp, b, s):
    m = host_p1_consts()
    m['xT'] = np.ascontiguousarray(inp['x'][b].T.reshape(8, 128, T).transpose(1, 0, 2))
    w = inp['w_in'][0]
    wm = np.stack([np.concatenate([w[:, 2056 + 128 * h:2056 + 128 * (h + 1)], w[:, 2568 + 128 * h:2568 + 128 * (h + 1)],
                                   w[:, 3080 + 128 * h:3080 + 128 * (h + 1)]], axis=1) for h in range(NH)])
    m['w_mb'] = np.ascontiguousarray(wm.reshape(NH, 8, 128, 384).transpose(0, 2, 1, 3))
    sel = np.zeros((128, 2), np.float32)
    sel[:, s] = 1.0
    m['sel'] = sel
    m['xT_own'] = np.ascontiguousarray(m['xT'][:, :, s * TOWN:(s + 1) * TOWN])
    m['c_cos_own'] = np.ascontiguousarray(m['c_cos'][:, s * TOWN:(s + 1) * TOWN])
    m['c_sin_own'] = np.ascontiguousarray(m['c_sin'][:, s * TOWN:(s + 1) * TOWN])
    cb = np.zeros((NT_OWN, NBLK), np.float32)
    vm = np.zeros((2, NT_OWN, NBLK), np.float32)
    for tt in range(NT_OWN):
        own = (NBLK // 2) * s + tt // 2
        cb[tt, own:] = -1e30
        vm[0, tt, :own] = 1.0
        vm[1, tt, own] = 1.0
    m['c_cbias_own'] = np.ascontiguousarray(np.broadcast_to(cb[None], (128, NT_OWN, NBLK)))
    m['c_vmask_own'] = np.ascontiguousarray(np.broadcast_to(vm[None], (128, 2, NT_OWN, NBLK)))
    m['sel_a'] = np.full((128, 1), 1.0 - s, np.float32)
    return m


def p1_consts_setup(nc, S, C, es):
    sb = lambda name, shape, dt: es.enter_context(nc.sbuf_tensor(name, shape, dt))
    C.xT = sb('xT_sb', [128, 8, T], BF16)
    C.sel = sb('sel_sb', [128, 2], F32)
    C.prot = sb('prot', [128, 128], BF16)
    C.caus = sb('caus', [128, 2, 256], BF16)
    C.cbias = sb('cbias', [128, NBLK, NBLK], F32)
    C.ones_bf = sb('ones_bf', [128, 128], BF16)
    S.add('sp', lambda e: e.dma_start(out=C.sel[:], in_=C.d_sel), writes=['sel'], ndma=1, semkey='k3')
    S.add('pool', lambda e: e.dma_start(out=C.prot[:], in_=C.d_prot), writes=['prot'], ndma=1, semkey='k4')
    S.add('pool', lambda e: e.dma_start(out=C.caus[:], in_=C.d_caus.rearrange("k p q -> p k q")), writes=['caus'], ndma=1, semkey='k5')
    S.add('sp', lambda e: e.dma_start(out=C.cbias[:], in_=C.d_cbias), writes=['cbias'], ndma=1, semkey='k6')
    S.add('dve', lambda e: e.memset(C.ones_bf[:], 1.0), writes=['ones'])


def build_moba(nc, S, C, es):
    sb = lambda name, shape, dt: es.enter_context(nc.sbuf_tensor(name, shape, dt))
    banks, bank_bf = C.banks, C.bank_bf
    NTL = T // 128
    HB_ = NBLK // 2
    w = [sb('wmb%d' % i, [128, 8, 384], BF16) for i in range(2)]
    xTo = sb('xTo_sb', [128, 8, TOWN], BF16)
    qT = sb('mqT', [128, TOWN], BF16)
    kT = sb('mkT', [128, T], BF16)
    vaug = sb('mv', [128, NTL, HD + 1], BF16)
    sel01 = sb('msel', [128, NT_OWN, NBLK], F32)
    cbo = sb('mcbo', [128, NT_OWN, NBLK], F32)
    vmo = sb('mvmo', [128, 2, NT_OWN, NBLK], F32)
    sela = sb('msela', [128, 1], F32)
    identA = sb('midentA', [128, 128], BF16)
    km32 = sb('km32', [128, NBLK], F32)
    kmb = sb('kmb', [128, NBLK], BF16)
    cs = [sb('cos%d' % i, [128, 512], F32) for i in range(2)]
    sn = [sb('sin%d' % i, [128, 512], F32) for i in range(2)]
    raw = [sb('raw%d' % i, [128, 512], BF16) for i in range(2)]
    ta = [sb('ta%d' % i, [128, 512], F32) for i in range(2)]
    tb = [sb('tb%d' % i, [128, 512], F32) for i in range(2)]
    gsm = [sb('gsm%d' % i, [128, 24], F32) for i in range(2)]
    PTs = [sb('PT%d' % i, [128, 512], BF16) for i in range(8)]
    Oacc = [sb('Oacc%d' % i, [128, HD + 1], F32) for i in range(8)]
    osm = [sb('mosm%d' % i, [128, 2], F32) for i in range(8)]
    ytk = [sb('mytk%d' % i, [128, HD], BF16) for i in range(8)]
    S.add('pool', lambda e: e.memset(vaug[:, :, HD:HD + 1], 1.0), writes=[('mvone',)])
    S.add('sp', lambda e: e.dma_start(out=cbo[:], in_=C.d_cbo), writes=['mcbo'], ndma=1, semkey='k11')
    S.add('sp', lambda e: e.dma_start(out=vmo[:], in_=C.d_vmo), writes=['mvmo'], ndma=1, semkey='k12')
    S.add('sp', lambda e: e.dma_start(out=sela[:], in_=C.d_sela), writes=['msela'], ndma=1, semkey='k13')
    _ts(S, 'dve', identA[:], C.ident_bf[:], sela[:, 0:1], None, ALU.mult, None, ['ident', 'msela'], ['midentA'])
    kk = [0]
    gk = 0

    def rope(W, rW, comp, dst, rdst, src, rsrc, tg, cosd, sind, cskey):
        tk = slice(tg * 512, (tg + 1) * 512)
        p = tg % 2
        S.add('sp', lambda e: [e.dma_start(out=cs[p][:], in_=cosd[:, tk]), e.dma_start(out=sn[p][:], in_=sind[:, tk])],
              writes=[('cs', p)], ndma=2, semkey=('cs', p))
        pb = kk[0] % 2
        bk = 6 + pb
        for c in range(8):
            _mm(S, banks[bk][:], W[:, c, comp * 128:(comp + 1) * 128], src[:, c, tk], c == 0, c == 7, [rW, (rsrc, tg)], [('bank', bk)])
        RAW, TA, TB = raw[pb], ta[pb], tb[pb]
        _act(S, RAW[:], banks[bk][:], AF.Copy, [('bank', bk)], [('raw', pb)])
        _mm(S, banks[bk][:], C.prot[:], RAW[:], True, True, [('raw', pb), 'prot'], [('bank', bk)])
        _tt(S, 'pool', TA[:], RAW[:], cs[p][:], ALU.mult, [('raw', pb), ('cs', p)], [('ta', pb)])
        _tt(S, 'dve', TB[:], banks[bk][:], sn[p][:], ALU.mult, [('bank', bk), ('cs', p)], [('tb', pb)])
        _tt(S, 'pool', dst[:, tk], TA[:], TB[:], ALU.add, [('ta', pb), ('tb', pb)], [(rdst, tg)])
        kk[0] += 1

    def attn(h, j, slot):
        qs = slice(j * 256, (j + 1) * 256)
        rq = ('mqT', j // 2)
        nblk = HB_ + j + 1
        for n in range(nblk):
            sbk = slot
            obk = 4 + slot
            pi = slot * 2 + (n % 2)
            Pt, rP = PTs[pi], ('PT', pi)
            for jj in range(2):
                kt = 2 * n + jj
                extra = (n == j) or (n == HB_ + j)
                _mm(S, banks[sbk][:, jj * 256:(jj + 1) * 256], kT[:, kt * 128:(kt + 1) * 128], qT[:, qs], True, not extra, [('mkT', kt // 4), rq], [('bank', sbk)])
                if n == j:
                    _mm(S, banks[sbk][:, jj * 256:(jj + 1) * 256], identA[:], C.caus[:, jj, :], False, True, ['midentA', 'caus'], [('bank', sbk)])
                elif n == HB_ + j:
                    _mm(S, banks[sbk][:, jj * 256:(jj + 1) * 256], C.ident_bf[:], C.caus[:, jj, :], False, True, ['ident', 'caus'], [('bank', sbk)])
            yield
            _act(S, Pt[:], banks[sbk][:], AF.Exp, [('bank', sbk)], [rP], scale=SCALE)
            yield
            for qt in range(2):
                for jj in range(2):
                    kt = 2 * n + jj
                    _mm(S, banks[obk][:, qt * 256:qt * 256 + HD + 1], Pt[:, jj * 256 + qt * 128:jj * 256 + (qt + 1) * 128], vaug[:, kt, :],
                        jj == 0, jj == 1, [rP, ('mv', kt // 4), ('mvone',)], [('bank', obk)])
            yield
            for qt in range(2):
                tt = 2 * j + qt
                OA, rOA = Oacc[slot * 2 + qt], ('Oacc', slot * 2 + qt)
                src = banks[obk][:, qt * 256:qt * 256 + HD + 1]
                sc_ = sel01[:, tt, n:n + 1]
                rds = [('bank', obk), ('msel', tt)]
                if n == 0:
                    _ts(S, 'dve', OA[:], src, sc_, None, ALU.mult, None, rds, [rOA])
                else:
                    _stt(S, OA[:], src, sc_, OA[:], ALU.mult, ALU.add, rds + [rOA], [rOA])
                yield
        for qt in range(2):
            tt = 2 * j + qt
            OA, rOA = Oacc[slot * 2 + qt], ('Oacc', slot * 2 + qt)
            OS, rOS = osm[slot * 2 + qt], ('mosm', slot * 2 + qt)
            YK, rYK = ytk[slot * 2 + qt], ('mytk', slot * 2 + qt)
            S.add('dve', lambda e, OS=OS, OA=OA: e.reciprocal(out=OS[:, 0:1], in_=OA[:, HD:HD + 1]), reads=[rOA], writes=[rOS])
            yield
            _ts(S, 'dve', YK[:], OA[:, 0:HD], OS[:, 0:1], None, ALU.mult, None, [rOA, rOS], [rYK])
            yield
            tbk = 4 + slot
            _tr(S, bank_bf[tbk][:, 0:128], YK[:], C.ident_bf[:], [rYK, 'ident'], [('bank', tbk)])
            _act(S, C.yT[:, 4 + h, tt * 128:(tt + 1) * 128], bank_bf[tbk][:, 0:128], AF.Copy, [('bank', tbk)], [('yT', 4 + h, tt)])
            yield

    for h in range(NH):
        W = w[h % 2]
        rW = ('wmb', h % 2)
        S.add('pool', lambda e, W=W, h=h: e.dma_start(out=W[:], in_=C.d_wmb[h], max_dma_last_dim=8192), writes=[rW], ndma=1, semkey=rW)
        if h == 0:
            for tg in range(T // 512):
                S.add('pool', lambda e, tg=tg: e.dma_start(out=C.xT[:, :, tg * 512:(tg + 1) * 512], in_=C.d_xT[:, :, tg * 512:(tg + 1) * 512]),
                      writes=[('xT', tg)], ndma=1, semkey=('xT', tg))
                if tg % 2 == 0:
                    S.add('pool', lambda e, tg=tg: e.dma_start(out=xTo[:, :, (tg // 2) * 512:(tg // 2 + 1) * 512], in_=C.d_xTo[:, :, (tg // 2) * 512:(tg // 2 + 1) * 512]),
                          writes=[('xTo', tg // 2)], ndma=1, semkey=('xTo', tg // 2))
        for tg in range(T // 512):
            rope(W, rW, 1, kT, 'mkT', C.xT, 'xT', tg, C.d_cos, C.d_sin, 0)
            if tg % 2 == 1:
                rope(W, rW, 0, qT, 'mqT', xTo, 'xTo', tg // 2, C.d_coso, C.d_sino, 1)
            for tl in range(4):
                tt = tg * 4 + tl
                for c in range(8):
                    _mm(S, banks[5][:, tl * 128:(tl + 1) * 128], C.xT[:, c, tt * 128:(tt + 1) * 128], W[:, c, 256:384], c == 0, c == 7, [rW, ('xT', tg)], [('bank', 5)])
            _act(S, vaug[:, tg * 4:(tg + 1) * 4, 0:HD], banks[5][:].rearrange("p (a d) -> p a d", a=4), AF.Copy, [('bank', 5)], [('mv', tg)])
        S.add('dve', lambda e: e.tensor_reduce(out=km32[:], in_=kT[:].rearrange("p (n k) -> p n k", k=256), axis=AX.X, op=ALU.add),
              reads=[('mkT', i) for i in range(8)], writes=['km32'])
        _act(S, kmb[:], km32[:], AF.Copy, ['km32'], ['kmb'], scale=1.0 / 256)
        for tt in range(NT_OWN):
            G = gsm[gk % 2]
            rG = ('gsm', gk % 2)
            gbk = 6 + (gk % 2)
            _mm(S, banks[gbk][:, 0:16], qT[:, tt * 128:(tt + 1) * 128], kmb[:], True, True, [('mqT', tt // 4), 'kmb'], [('bank', gbk)])
            _tt(S, 'dve', G[:, 0:16], banks[gbk][:, 0:16], cbo[:, tt, :], ALU.add, [('bank', gbk), 'mcbo'], [(rG, 'g')])
            S.add('dve', lambda e, G=G: e.max(out=G[:, 16:24], in_=G[:, 0:16]), reads=[(rG, 'g')], writes=[(rG, 't')])
            _ts(S, 'dve', G[:, 0:16], G[:, 0:16], G[:, 18:19], None, ALU.is_ge, None, [(rG, 'g'), (rG, 't')], [(rG, 'g')])
            _tt(S, 'dve', G[:, 0:16], G[:, 0:16], vmo[:, 0, tt, :], ALU.mult, [(rG, 'g'), 'mvmo'], [(rG, 'g')])
            _tt(S, 'dve', sel01[:, tt, :], G[:, 0:16], vmo[:, 1, tt, :], ALU.add, [(rG, 'g'), 'mvmo'], [('msel', tt)])
            gk += 1
        def lane(h, js, slot):
            for j in js:
                yield from attn(h, j, slot)
        _interleave([lane(h, js, i) for i, js in enumerate(((7, 0), (6, 1), (5, 2), (4, 3)))])


def build_p1_test(which='moba'):
    nc = bass.Bass("TRN2", target_bir_lowering=False)
    C = Ctx()
    declare_common_dram(nc, C)
    declare_p1_dram(nc, C)
    declare_dn_dram(nc, C)
    d_yo = nc.dram_tensor('yT_out', [128, 8, TOWN], BF16, kind="ExternalOutput").ap()
    with contextlib.ExitStack() as es:
        sb = lambda name, shape, dt: es.enter_context(nc.sbuf_tensor(name, shape, dt))
        C.banks = _psum_banks(nc, es)
        C.bank_bf = [b[:].bitcast(BF16) for b in C.banks]
        C.yT = sb('yT', [128, 8, TOWN], BF16)
        P = SemPool(nc, es)
        with contextlib.ExitStack() as es1:
            S0 = Sched(nc, P)
            consts_setup(nc, S0, C, es)
            p1_consts_setup(nc, S0, C, es1)
            S0.add('pool', lambda e: e.memset(C.yT[:], 0.0), writes=['yT'])
            if which == 'moba':
                build_moba(nc, S0, C, es1)
            if which == 'dn':
                S0.add('pool', lambda e: [e.dma_start(out=C.xT[:, c, :], in_=C.d_xT[:, c, :], max_dma_last_dim=8192) for c in range(8)],
                       writes=[('xT', c) for c in range(8)], ndma=8, semkey='xT')
                build_dn(nc, S0, C, es1)
            S0.emit()
            nc.all_engine_barrier()
            S1 = Sched(nc, P)
            S1.add('sp', lambda e: e.dma_start(out=d_yo, in_=C.yT[:]), writes=['yo'], ndma=1, semkey='yo')
            S1.emit()
    return nc


def _mm(S, out, lhsT, rhs, start, stop, reads, writes):
    return S.add('pe', lambda e: e.matmul(out, lhsT=lhsT, rhs=rhs, start=start, stop=stop), reads=reads, writes=writes)


def _tr(S, out, in_, ident, reads, writes):
    return S.add('pe', lambda e: e.transpose(out, in_, ident), reads=reads, writes=writes)


def _act(S, out, in_, func, reads, writes, bias=None, scale=1.0, accum_out=None):
    kw = {}
    if bias is not None:
        kw['bias'] = bias
    if accum_out is not None:
        kw['accum_out'] = accum_out
    return S.add('act', lambda e: e.activation(out=out, in_=in_, func=func, scale=scale, **kw), reads=reads, writes=writes)


def _tt(S, eng, out, in0, in1, op, reads, writes):
    return S.add(eng, lambda e: e.tensor_tensor(out=out, in0=in0, in1=in1, op=op), reads=reads, writes=writes)


def _ts(S, eng, out, in0, s1, s2, op0, op1, reads, writes):
    if s2 is None and eng == 'pool' and op0 == ALU.mult:
        s2, op1 = 1.0, ALU.mult
    if s2 is None:
        return S.add(eng, lambda e: e.tensor_scalar(out=out, in0=in0, scalar1=s1, scalar2=None, op0=op0), reads=reads, writes=writes)
    return S.add(eng, lambda e: e.tensor_scalar(out=out, in0=in0, scalar1=s1, scalar2=s2, op0=op0, op1=op1), reads=reads, writes=writes)


def _stt(S, out, in0, scalar, in1, op0, op1, reads, writes):
    return S.add('dve', lambda e: e.scalar_tensor_tensor(out=out, in0=in0, scalar=scalar, in1=in1, op0=op0, op1=op1), reads=reads, writes=writes)


class RR:
    def __init__(self, tiles, name):
        self.tiles = tiles
        self.name = name
        self.i = 0

    def get(self):
        k = self.i % len(self.tiles)
        self.i += 1
        return self.tiles[k], (self.name, k)


def declare_dn_dram(nc, C):
    di = lambda name, shape: nc.dram_tensor(name, shape, F32, kind="ExternalInput").ap()
    C.d_wdn = di('w_dn', [NH, 128, 8, 514])
    C.d_cw = di('c_w', [NH, 128, 3, 4])
    C.d_adt = di('a_dt', [128, 8])
    C.d_nw = di('n_w', [128, 128])
    C.d_masks = di('c_masks', [6, 128, 128])


def host_dn(inp):
    m = {}
    w = inp['w_in'][0]
    wd = np.stack([np.concatenate([w[:, 128 * h:128 * (h + 1)], w[:, 512 + 128 * h:512 + 128 * (h + 1)],
                                   w[:, 1024 + 128 * h:1024 + 128 * (h + 1)], w[:, 1536 + 128 * h:1536 + 128 * (h + 1)],
                                   w[:, 2048 + h:2049 + h], w[:, 2052 + h:2053 + h]], axis=1) for h in range(NH)])
    m['w_dn'] = np.ascontiguousarray(wd.reshape(NH, 8, 128, 514).transpose(0, 2, 1, 3))
    cw = inp['conv_w'][0]
    m['c_w'] = np.ascontiguousarray(np.stack([np.stack([cw[:, comp * 512 + h * 128: comp * 512 + (h + 1) * 128].T for comp in range(3)], axis=1)
                                              for h in range(NH)]))
    adt = np.concatenate([inp['a_log'][0], inp['dt_bias'][0]])
    m['a_dt'] = np.ascontiguousarray(np.broadcast_to(adt[None, :], (128, 8)))
    m['n_w'] = np.ascontiguousarray(np.broadcast_to(inp['dn_norm_w'][0][None, :], (128, 128)))
    idx = np.arange(128)
    same = (idx[:, None] // 64) == (idx[None, :] // 64)
    M1 = (same & (idx[:, None] <= idx[None, :])).astype(np.float32)
    M2 = (same & (idx[:, None] > idx[None, :])).astype(np.float32)
    MC0 = np.broadcast_to((idx[:, None] < 64), (128, 128)).astype(np.float32)
    MC1 = np.broadcast_to((idx[:, None] >= 64), (128, 128)).astype(np.float32)
    strict = (same & (idx[:, None] > idx[None, :])).astype(np.float32)
    incl = (same & (idx[:, None] >= idx[None, :])).astype(np.float32)
    m['c_masks'] = np.ascontiguousarray(np.stack([M1, M2, MC0, MC1, strict, incl]))
    return m


DBG = {'heads': NH, 'tiles': T // 128, 'stage': 9, 'neumann': 5}


def _interleave(gens, weights=None, offsets=None):
    gens = list(gens)
    weights = list(weights) if weights is not None else [1] * len(gens)
    offsets = list(offsets) if offsets is not None else [0] * len(gens)
    live = list(zip(gens, weights, offsets))
    rnd = 0
    while live:
        nxt = []
        for g, wgt, off in live:
            alive = True
            if rnd >= off:
                for _ in range(wgt):
                    try:
                        next(g)
                    except StopIteration:
                        alive = False
                        break
            if alive:
                nxt.append((g, wgt, off))
        live = nxt
        rnd += 1


def _rolling(thunks, width, skew):
    thunks = list(thunks)
    live = []
    rnd = 0
    last_start = -skew
    while thunks or live:
        if thunks and len(live) < width and rnd - last_start >= skew:
            live.append(thunks.pop(0)())
            last_start = rnd
        nxt = []
        for g in live:
            try:
                next(g)
                nxt.append(g)
            except StopIteration:
                pass
        live = nxt
        rnd += 1


def build_dn(nc, S, C, es):
    sb = lambda name, shape, dt: es.enter_context(nc.sbuf_tensor(name, shape, dt))
    banks, bank_bf = C.banks, C.bank_bf
    NTL = T // 128
    NS = 8
    GRP = 4
    masks = sb('dmasks', [128, 6, 128], F32)
    cw = sb('dcw', [128, NH, 3, 4], F32)
    adt = sb('dadt', [128, 8], F32)
    nA = sb('dnA', [128, 4], F32)
    nw = sb('dnw', [128, 128], F32)
    w = [sb('wdn%d' % i, [128, 8, 514], BF16) for i in range(1)] * 2
    qnT = sb('dqnT', [128, T], BF16)
    knT = sb('dknT', [128, T], BF16)
    vcT = sb('dvcT', [128, T], BF16)
    zs = sb('dzs', [128, NTL, 128], BF16)
    bg = sb('dbg', [128, NTL, 2], F32)
    sc = sb('dsc', [128, 12, NTL], F32)
    rawb = [[sb('draw%d_%d' % (c, i), [128, 515], BF16) for i in range(2)] for c in range(3)]
    dgw = sb('ddgw', [128, 3, 4, 128], BF16)
    qcs = [[sb('dqc%d_%d' % (c, i), [128, 512], BF16) for i in range(2)] for c in range(2)]
    sqs = [sb('dsq%d' % c, [128, 512], BF16) for c in range(2)]
    rns = [sb('drn%d' % c, [128, 512], F32) for c in range(2)]
    f32p = [RR([sb('df%d_%d' % (g, i), [128, 128], F32) for i in range(4)], 'df%d' % g) for g in range(GRP)]
    b16p = [RR([sb('db%d_%d' % (g, i), [128, 128], BF16) for i in range(5)], 'db%d' % g) for g in range(GRP)]
    abp = [RR([sb('dab%d_%d' % (g, i), [128, 256], BF16) for i in range(3)], 'dab%d' % g) for g in range(GRP)]
    f32s = RR([sb('dfs%d' % i, [128, 128], F32) for i in range(2)], 'dfs')
    XWs = [sb('dXW%d' % i, [128, 128], BF16) for i in range(GRP)]
    VBs = [sb('dVB%d' % i, [128, 128], BF16) for i in range(GRP)]
    DGs = [sb('dDG%d' % i, [128, 128], BF16) for i in range(GRP)]
    WT = [sb('dWT%d' % i, [128, 128], BF16) for i in range(NS)]
    BQs = [sb('dBQ%d' % i, [128, 256], BF16) for i in range(NS)]
    KD = [sb('dKD%d' % i, [128, 128], BF16) for i in range(NS)]
    QD = [sb('dQD%d' % i, [128, 128], BF16) for i in range(NS)]
    U = [sb('dU%d' % i, [128, 128], F32) for i in range(NS)]
    vnew = [sb('dvn%d' % i, [128, 128], BF16) for i in range(2)]
    otok = [sb('dot%d' % i, [128, 128], F32) for i in range(2)]
    ytok = [sb('dyt%d' % i, [128, 128], BF16) for i in range(2)]
    osm = [sb('dosm%d' % i, [128, 4], F32) for i in range(2)]
    S32 = sb('dS32', [128, 128], F32)
    Sbf = sb('dSbf', [128, 128], BF16)
    gbi = [0]
    GB = [2, 3, 4, 5, 0]

    def gbank():
        k = GB[gbi[0] % len(GB)]
        gbi[0] += 1
        return k

    S.add('sp', lambda e: e.dma_start(out=masks[:], in_=C.d_masks.rearrange("m p q -> p m q")), writes=['dmasks'], ndma=1, semkey='k7')
    S.add('sp', lambda e: e.dma_start(out=cw[:], in_=C.d_cw.rearrange("h p c k -> p h c k")), writes=['dcw'], ndma=1, semkey='k8')
    S.add('sp', lambda e: e.dma_start(out=adt[:], in_=C.d_adt), writes=['dadt'], ndma=1, semkey='k9')
    S.add('sp', lambda e: e.dma_start(out=nw[:], in_=C.d_nw), writes=['dnw'], ndma=1, semkey='k10')
    _act(S, nA[:], adt[:, 0:4], AF.Exp, ['dadt'], ['dnA0'])
    _ts(S, 'dve', nA[:], nA[:], -1.0, None, ALU.mult, None, ['dnA0'], ['dnA'])
    M1, M2, MC0, MC1, MST, MIN = (masks[:, i, :] for i in range(6))

    def prep(h, t, sl):
        f32t, b16t = f32p[t % GRP], b16p[t % GRP]
        ts_ = slice(t * 128, (t + 1) * 128)
        rq, rk, rv = ('dqnT', t // 4), ('dknT', t // 4), ('dvcT', t // 4)
        GM, rGM = f32t.get()
        _ts(S, 'pool', GM[:], M2, sc[:, 2, t:t + 1], None, ALU.mult, None, ['dmasks', 'dg'], [rGM]); yield
        b1 = gbank()
        _mm(S, banks[b1][:, 0:128], M1, GM[:], True, True, ['dmasks', rGM], [('bank', b1)])
        _mm(S, banks[b1][:, 128:256], knT[:, ts_], knT[:, ts_], True, True, [rk], [('bank', b1)])
        _mm(S, banks[b1][:, 256:384], qnT[:, ts_], knT[:, ts_], True, True, [rk, rq], [('bank', b1)]); yield
        Dm, rD = f32t.get()
        _act(S, Dm[:], banks[b1][:, 0:128], AF.Exp, [('bank', b1)], [rD]); yield
        A1, rA1 = f32t.get()
        _stt(S, A1[:], banks[b1][:, 128:256], sc[:, 0, t:t + 1], Dm[:], ALU.mult, ALU.mult, [('bank', b1), rD, 'dbeta'], [rA1]); yield
        Q1, rQ1 = f32t.get()
        _tt(S, 'dve', Q1[:], banks[b1][:, 256:384], Dm[:], ALU.mult, [('bank', b1), rD], [rQ1]); yield
        A, rA = b16t.get()
        _tt(S, 'pool', A[:], A1[:], MST, ALU.mult, [rA1, 'dmasks'], [rA]); yield
        QK, rQK = b16t.get()
        _tt(S, 'pool', QK[:], Q1[:], MIN, ALU.mult, [rQ1, 'dmasks'], [rQK]); yield
        b3 = gbank()
        _tr(S, bank_bf[b3][:, 0:128], A[:], C.ident_bf[:], [rA, 'ident'], [('bank', b3)])
        _tr(S, bank_bf[b3][:, 128:256], QK[:], C.ident_bf[:], [rQK, 'ident'], [('bank', b3)])
        _tr(S, bank_bf[b3][:, 256:384], knT[:, ts_], C.ident_bf[:], [rk, 'ident'], [('bank', b3)])
        _tr(S, bank_bf[b3][:, 384:512], vcT[:, ts_], C.ident_bf[:], [rv, 'ident'], [('bank', b3)]); yield
        BQ, rB = BQs[sl], ('dBQ', sl)
        B = BQ[:, 0:128]
        _act(S, BQ[:], bank_bf[b3][:, 0:256], AF.Copy, [('bank', b3)], [rB]); yield
        XW, rXW = XWs[t % GRP], ('dXW', t % GRP)
        _ts(S, 'dve', XW[:], bank_bf[b3][:, 256:384], sc[:, 3, t:t + 1], None, ALU.mult, None, [('bank', b3), 'dbw'], [rXW]); yield
        _act(S, KD[sl][:], bank_bf[b3][:, 256:384], AF.Copy, [('bank', b3), 'dexp'], [('dKD', sl)], scale=sc[:, 5, t:t + 1]); yield
        VB, rVB = VBs[t % GRP], ('dVB', t % GRP)
        _ts(S, 'dve', VB[:], bank_bf[b3][:, 384:512], sc[:, 0, t:t + 1], None, ALU.mult, None, [('bank', b3), 'dbeta'], [rVB]); yield
        Y, rY = b16t.get()
        _tt(S, 'pool', Y[:], C.ident_bf[:], B, ALU.subtract, ['ident', rB], [rY]); yield
        DG, rDG = DGs[t % GRP], ('dDG', t % GRP)
        _ts(S, 'pool', DG[:], C.ident_bf[:], sc[:, 4, t:t + 1], None, ALU.mult, None, ['ident', 'dexp'], [rDG]); yield
        Ak, rAk, Bk, rBk = A[:], rA, B, rB
        for k in range(DBG['neumann']):
            b4 = gbank()
            _mm(S, banks[b4][:, 0:128], Bk, Ak, True, True, [rAk, rBk], [('bank', b4)])
            if k < 4:
                _mm(S, banks[b4][:, 128:256], Ak, Bk, True, True, [rAk, rBk], [('bank', b4)])
            yield
            AB, rAB = abp[t % GRP].get()
            nc_ = 256 if k < 4 else 128
            _act(S, AB[:, 0:nc_], banks[b4][:, 0:nc_], AF.Copy, [('bank', b4)], [rAB]); yield
            A2, rA2, B2, rB2 = AB[:, 0:128], rAB, AB[:, 128:256], rAB
            b5 = gbank()
            _mm(S, banks[b5][:, 0:128], A2, Y[:], True, True, [rA2, rY], [('bank', b5)]); yield
            Y2, rY2 = b16t.get()
            _tt(S, 'dve', Y2[:], banks[b5][:, 0:128], Y[:], ALU.add, [('bank', b5), rY], [rY2]); yield
            Y, rY = Y2, rY2
            if k < 4:
                Ak, rAk, Bk, rBk = A2, rA2, B2, rB2
        TT, rTT = Y, rY
        b7 = gbank()
        _mm(S, banks[b7][:, 0:128], XW[:], TT[:], True, True, [rXW, rTT], [('bank', b7)])
        _mm(S, banks[b7][:, 128:256], TT[:], VB[:], True, True, [rVB, rTT], [('bank', b7)])
        _mm(S, banks[b7][:, 256:384], C.ones_bf[:], DG[:], True, True, [rDG, 'ones'], [('bank', b7)]); yield
        _act(S, WT[sl][:], banks[b7][:, 0:128], AF.Copy, [('bank', b7)], [('dWT', sl)]); yield
        _ts(S, 'dve', U[sl][:], banks[b7][:, 128:256], 1.0, None, ALU.mult, None, [('bank', b7)], [('dU', sl)]); yield
        _tt(S, 'dve', QD[sl][:], banks[b7][:, 256:384], qnT[:, ts_], ALU.mult, [('bank', b7), rq], [('dQD', sl)]); yield

    def scan(h, t, sl):
        pp = t % 2
        obk = 7 if pp == 0 else 1
        for c in range(2):
            rows = slice(c * 64, (c + 1) * 64)
            _mm(S, banks[6][rows, 0:128], WT[sl][:, rows], Sbf[:], True, True, [('dWT', sl), 'dSbf'], [('bank', 6)]); yield
            _tt(S, 'dve', vnew[pp][rows, :], U[sl][rows, :], banks[6][rows, 0:128], ALU.subtract, [('dU', sl), ('bank', 6)], [('dvn', pp, c)]); yield
            _mm(S, banks[6][:, 128:256], KD[sl][rows, :], vnew[pp][rows, :], True, True, [('dKD', sl), ('dvn', pp, c)], [('bank', 6)])
            _mm(S, banks[obk][rows, 0:128], QD[sl][:, rows], Sbf[:], True, False, [('dQD', sl), 'dSbf'], [('bank', obk)])
            _mm(S, banks[obk][rows, 0:128], BQs[sl][rows, 128 + c * 64:128 + (c + 1) * 64], vnew[pp][rows, :], False, True, [('dBQ', sl), ('dvn', pp, c)], [('bank', obk)]); yield
            _stt(S, Sbf[:], S32[:], sc[:, 6 + c, t:t + 1], banks[6][:, 128:256], ALU.mult, ALU.add, ['dS32', 'dexp', ('bank', 6)], ['dSbf']); yield
            _stt(S, S32[:], S32[:], sc[:, 6 + c, t:t + 1], banks[6][:, 128:256], ALU.mult, ALU.add, ['dS32', 'dexp', ('bank', 6)], ['dS32']); yield
        _act(S, otok[pp][:], banks[obk][:, 0:128], AF.Copy, [('bank', obk)], [('dot', pp)]); yield
        OS = osm[pp]
        ro = [('dot', pp)]
        J, rJ = f32s.get()
        _act(S, J[:], otok[pp][:], AF.Square, ro, [rJ, ('dosm', pp, 0)], accum_out=OS[:, 0:1]); yield
        _act(S, OS[:, 1:2], OS[:, 0:1], AF.Ln, [('dosm', pp, 0), 'eps'], [('dosm', pp, 1)], scale=1.0 / 128, bias=C.eps[:, 1:2]); yield
        _act(S, OS[:, 2:3], OS[:, 1:2], AF.Exp, [('dosm', pp, 1)], [('dosm', pp, 2)], scale=-0.5); yield
        _stt(S, ytok[pp][:], otok[pp][:], OS[:, 2:3], zs[:, t, :], ALU.mult, ALU.mult, ro + [('dosm', pp, 2), ('dzs', t)], [('dyt', pp)]); yield
        b9 = gbank()
        _tr(S, bank_bf[b9][:, 0:128], ytok[pp][:], C.ident_bf[:], [('dyt', pp), 'ident'], [('bank', b9)]); yield
        dq = slice((t % 16) * 128, (t % 16 + 1) * 128)
        rYo = ('yT', h, t % 16)
        if t < 16:
            _ts(S, 'dve', C.yT[:, h, dq], bank_bf[b9][:, 0:128], C.sel[:, 0:1], None, ALU.mult, None, [('bank', b9), 'sel'], [rYo])
        else:
            _stt(S, C.yT[:, h, dq], bank_bf[b9][:, 0:128], C.sel[:, 1:2], C.yT[:, h, dq], ALU.mult, ALU.add, [('bank', b9), 'sel', rYo], [rYo])
        yield

    def scans(h, tiles):
        for t in tiles:
            yield from scan(h, t, t % NS)

    for h in range(DBG['heads']):
        W = w[h % 2]
        rW = ('wdn', 0)
        S.add('pool', lambda e, W=W, h=h: e.dma_start(out=W[:], in_=C.d_wdn[h], max_dma_last_dim=2056), writes=[rW], ndma=1, semkey=rW)
        for comp in range(3):
            for k in range(4):
                _ts(S, 'dve' if (comp + k) % 2 else 'pool', dgw[:, comp, k, :], C.ident_bf[:], cw[:, h, comp, k:k + 1], None, ALU.mult, None,
                    ['ident', 'dcw'], [('ddgw', comp)])
            S.add('pool', lambda e, comp=comp: e.memset(rawb[comp][0][:, 0:3], 0.0), writes=[('draw', comp, 0, 'h')])
        for tg in range(T // 512):
            tk = slice(tg * 512, (tg + 1) * 512)
            p = tg % 2
            for comp in range(3):
                for c in range(8):
                    _mm(S, banks[comp][:], W[:, c, comp * 128:(comp + 1) * 128], C.xT[:, c, tk], c == 0, c == 7, [rW, ('xT', c)], [('bank', comp)])
            for comp in range(3):
                RAW = rawb[comp][p]
                _act(S, RAW[:, 3:515], banks[comp][:], AF.Copy, [('bank', comp)], [('draw', comp, p, 'm')])
                if tg < 7:
                    S.add('pool', lambda e, comp=comp, p=p, RAW=RAW: e.tensor_copy(out=rawb[comp][1 - p][:, 0:3], in_=RAW[:, 512:515]),
                          reads=[('draw', comp, p, 'm')], writes=[('draw', comp, 1 - p, 'h')])
            for comp in range(3):
                RAW = rawb[comp][p]
                for k in range(4):
                    _mm(S, banks[comp][:], dgw[:, comp, k, :], RAW[:, k:k + 512], k == 0, k == 3,
                        [('ddgw', comp), ('draw', comp, p, 'm'), ('draw', comp, p, 'h')], [('bank', comp)])
            for t in range(tg * 4, tg * 4 + 4):
                bk = 3 + (t % 2)
                for c in range(8):
                    _mm(S, banks[bk][:, 0:130], C.xT[:, c, t * 128:(t + 1) * 128], W[:, c, 384:514], c == 0, c == 7, [rW, ('xT', c)], [('bank', bk)])
                _act(S, zs[:, t, :], banks[bk][:, 0:128], AF.Silu, [('bank', bk)], [('dzs', t)])
                _ts(S, 'dve', bg[:, t, :], banks[bk][:, 128:130], 1.0, None, ALU.mult, None, [('bank', bk)], [('dbg', t)])
            _act(S, vcT[:, tk], banks[2][:], AF.Silu, [('bank', 2)], [('dvcT', tg)])
            for comp in range(2):
                _act(S, qcs[comp][p][:], banks[comp][:], AF.Silu, [('bank', comp)], [('dqc', comp, p)])
            for comp in range(2):
                _tt(S, 'pool', sqs[comp][:], qcs[comp][p][:], qcs[comp][p][:], ALU.mult, [('dqc', comp, p)], [('dsq', comp)])
                _mm(S, banks[5 + comp][:], C.ones_bf[:], sqs[comp][:], True, True, [('dsq', comp), 'ones'], [('bank', 5 + comp)])
            for comp in range(2):
                _act(S, rns[comp][:], banks[5 + comp][:], AF.Ln, [('bank', 5 + comp), 'eps'], [('drn', comp)], bias=C.eps[:, 1:2])
            for comp in range(2):
                _act(S, rns[comp][:], rns[comp][:], AF.Exp, [('drn', comp)], [('drn', comp)], scale=-0.5, bias=(C.eps[:, 2:3] if comp == 0 else None))
                dst, rdst = (qnT, 'dqnT') if comp == 0 else (knT, 'dknT')
                _tt(S, 'dve', dst[:, tk], qcs[comp][p][:], rns[comp][:], ALU.mult, [('dqc', comp, p), ('drn', comp)], [(rdst, tg)])
        if DBG['stage'] < 2:
            continue
        allbg = [('dbg', t) for t in range(NTL)]
        S.add('pool', lambda e: e.tensor_tensor(out=zs[:], in0=zs[:], in1=nw[:].unsqueeze(1).to_broadcast([128, NTL, 128]), op=ALU.mult),
              reads=[('dzs', t) for t in range(NTL)] + ['dnw'], writes=[('dzs', t) for t in range(NTL)])
        _act(S, sc[:, 1, :], bg[:, :, 0], AF.Exp, allbg, ['dsc1'], scale=-1.0)
        _ts(S, 'dve', sc[:, 1, :], sc[:, 1, :], 1.0, None, ALU.add, None, ['dsc1'], ['dsc1'])
        S.add('dve', lambda e: e.reciprocal(out=sc[:, 0, :], in_=sc[:, 1, :]), reads=['dsc1'], writes=['dbeta'])
        _act(S, sc[:, 8, :], bg[:, :, 1], AF.Exp, allbg + ['dadt'], ['dsc8'], bias=adt[:, 4 + h:5 + h])
        _act(S, sc[:, 8, :], sc[:, 8, :], AF.Ln, ['dsc8', 'eps'], ['dsc8'], bias=C.eps[:, 3:4])
        _ts(S, 'dve', sc[:, 2, :], sc[:, 8, :], nA[:, h:h + 1], None, ALU.mult, None, ['dsc8', 'dnA'], ['dg'])
        cb = 0
        for i, Mx in enumerate((M1, M2, MC0, MC1)):
            _mm(S, banks[cb][:, i * 32:(i + 1) * 32], Mx, sc[:, 2, :], True, True, ['dmasks', 'dg'], [('bank', cb)])
        _act(S, sc[:, 4:8, :], banks[cb][:, 0:128].rearrange("p (a t) -> p a t", a=4), AF.Exp, [('bank', cb)], ['dexp'])
        _tt(S, 'dve', sc[:, 3, :], sc[:, 0, :], sc[:, 4, :], ALU.mult, ['dbeta', 'dexp'], ['dbw'])
        S.add('pool', lambda e: e.memset(S32[:], 0.0), writes=['dS32'])
        S.add('pool', lambda e: e.memset(Sbf[:], 0.0), writes=['dSbf'])
        if DBG['stage'] < 3:
            continue
        ntl = DBG['tiles']
        groups = [list(range(g, min(g + GRP, ntl))) for g in range(0, ntl, GRP)]
        prev = None
        for grp in groups:
            gens = [prep(h, t, t % NS) for t in grp]
            wts = [1] * len(gens)
            if prev is not None:
                gens.append(scans(h, prev))
                wts.append(2)
            _interleave(gens, wts)
            prev = grp
        _interleave([scans(h, prev)])


def build_full():
    nc = bass.Bass("TRN2", target_bir_lowering=False)
    C = Ctx()
    declare_common_dram(nc, C)
    declare_p1_dram(nc, C)
    declare_dn_dram(nc, C)
    with contextlib.ExitStack() as es:
        sb = lambda name, shape, dt: es.enter_context(nc.sbuf_tensor(name, shape, dt))
        C.banks = _psum_banks(nc, es)
        C.bank_bf = [b[:].bitcast(BF16) for b in C.banks]
        C.yT = sb('yT', [128, 8, TOWN], BF16)
        P = SemPool(nc, es)
        with contextlib.ExitStack() as es1:
            with contextlib.ExitStack() as esm:
                Sm = Sched(nc, P)
                consts_setup(nc, Sm, C, es)
                p1_consts_setup(nc, Sm, C, es1)
                build_moba(nc, Sm, C, esm)
                Sm.add('pool', lambda e: e.memset(C.yT[:, 0:4, :], 0.0), writes=[('yT', c, i) for c in range(4) for i in range(16)])
                Sm.emit()
            nc.all_engine_barrier()
            with contextlib.ExitStack() as esd:
                Sd = Sched(nc, P)
                build_dn(nc, Sd, C, esd)
                Sd.emit()
            nc.all_engine_barrier()
        with contextlib.ExitStack() as es2:
            sb2 = lambda name, shape, dt: es2.enter_context(nc.sbuf_tensor(name, shape, dt))
            C.ysb = sb2('ysb', [128, NT_OWN, D], F32)
            C.hT = sb2('hT', [128, 8, TOWN], BF16)
            C.gatesT = sb2('gatesT', [16, TOWN], BF16)
            with contextlib.ExitStack() as es3:
                S1 = Sched(nc, P)
                build_p2a(nc, S1, C, es3)
                S1.emit()
            nc.all_engine_barrier()
            with contextlib.ExitStack() as es4:
                S2 = Sched(nc, P)
                build_p2b(nc, S2, C, es4)
                S2.emit()
    return nc


_NC_CACHE = [None]


def kernel(**inputs):
    inp = {k: np.asarray(v) for k, v in inputs.items()}
    if _NC_CACHE[0] is None:
        _NC_CACHE[0] = build_full()
    nc = _NC_CACHE[0]
    dn = host_dn(inp)
    maps = []
    for c in range(8):
        b, s = c // 2, c % 2
        m = host_common(inp, b, s)
        m.update(host_p1(inp, b, s))
        m.update(dn)
        maps.append(m)
    res = run_bass_kernel_spmd(nc, maps, core_ids=list(range(8)))
    out = np.empty((4, T, D), np.float32)
    for c in range(8):
        b, s = c // 2, c % 2
        out[b, s * TOWN:(s + 1) * TOWN, :] = np.asarray(res.results[c]['out'])
    return out
```

```python
import contextlib
import numpy as np
import concourse.bass as bass
import concourse.mybir as mybir
from concourse.bass_utils import run_bass_kernel_spmd

AF = mybir.ActivationFunctionType
ALU = mybir.AluOpType
AX = mybir.AxisListType
F32 = mybir.dt.float32
BF16 = mybir.dt.bfloat16

ENGS = ['pe', 'act', 'dve', 'pool', 'sp']

D = 1024
T = 4096
TOWN = 2048
NT_OWN = TOWN // 128
ALPHA = 2.0 ** 0.25
LN_EPS = 1e-5
NEXP = 16
DEXP = 256
BIG = 30000.0


class _Op:
    __slots__ = ('eng', 'idx', 'fn', 'deps', 'ms', 'cnt', 'ndma', 'semkey', 'dmaval')

    def __init__(self, eng, idx, fn, ndma, semkey):
        self.eng = eng
        self.idx = idx
        self.fn = fn
        self.deps = []
        self.ms = False
        self.cnt = 0
        self.ndma = ndma
        self.semkey = semkey
        self.dmaval = 0


class SemPool:
    def __init__(self, nc, es):
        self.nc = nc
        self.es = es
        self.engsem = {e: es.enter_context(nc.semaphore('s_' + e)) for e in ENGS}
        self.engbase = {e: 0 for e in ENGS}
        self.dmasem = {}
        self.dma_tot = {}

    def dsem(self, key):
        if key not in self.dmasem:
            self.dmasem[key] = self.es.enter_context(self.nc.semaphore('d%d' % len(self.dmasem)))
            self.dma_tot[key] = 0
        return self.dmasem[key]


ATTACH_WAIT = True


class Sched:
    def __init__(self, nc, pool):
        self.nc = nc
        self.pool = pool
        self.ops = {e: [] for e in ENGS}
        self.lastw = {}
        self.readers = {}
        self.used_dma = set()

    LIMIT = [10 ** 9]
    COUNT = [0]

    def add(self, eng, fn, reads=(), writes=(), ndma=0, semkey=None):
        Sched.COUNT[0] += 1
        if Sched.COUNT[0] > Sched.LIMIT[0] and not (semkey == 'yo'):
            return None
        lst = self.ops[eng]
        op = _Op(eng, len(lst), fn, ndma, semkey)
        if ndma:
            assert semkey is not None
            self.pool.dsem(semkey)
            self.pool.dma_tot[semkey] += 16 * ndma
            op.dmaval = self.pool.dma_tot[semkey]
            self.used_dma.add(semkey)
        deps = {}
        for r in reads:
            w = self.lastw.get(r)
            if w is not None:
                deps[id(w)] = (w, True)
            if isinstance(r, tuple) and r[0] == 'bank':
                for rd in self.readers.get(r, ()):
                    if rd.eng != eng and id(rd) not in deps:
                        deps[id(rd)] = (rd, True)
        for r in writes:
            w = self.lastw.get(r)
            if w is not None and id(w) not in deps:
                deps[id(w)] = (w, False)
            for rd in self.readers.get(r, ()):
                if id(rd) not in deps:
                    deps[id(rd)] = (rd, False)
        for d, raw in deps.values():
            if d is op:
                continue
            if d.eng == eng and not d.ndma and eng == 'pe':
                continue
            op.deps.append(d)
            if not d.ndma:
                d.ms = True
        for r in reads:
            self.readers.setdefault(r, []).append(op)
        for r in writes:
            self.lastw[r] = op
            self.readers[r] = []
        lst.append(op)
        return op

    def emit(self):
        nc = self.nc
        pool = self.pool
        engsem, dmasem = pool.engsem, pool.dmasem
        for e in ENGS:
            cands = [o for o in self.ops[e] if not o.ndma]
            if e != 'sp' and cands:
                cands[-1].ms = True
        for e in ENGS:
            c = pool.engbase[e]
            for op in self.ops[e]:
                if op.ms and not op.ndma:
                    c += 1
                op.cnt = c
            pool.engbase[e] = c
        with nc.Block() as block:
            def run(ename, eng):
                waited = {}
                for op in self.ops[ename]:
                    need = {}
                    for d in op.deps:
                        if d.ndma:
                            s, v = dmasem[d.semkey], d.dmaval
                        else:
                            s, v = engsem[d.eng], d.cnt
                        key = id(s)
                        if key not in need or need[key][1] < v:
                            need[key] = (s, v)
                    pend = [(key, s, v) for key, (s, v) in need.items() if waited.get(key, 0) < v]
                    for key, s, v in pend:
                        waited[key] = v
                    attach = None
                    if ATTACH_WAIT and len(pend) >= 1 and not op.ndma:
                        attach = pend.pop()
                    for key, s, v in pend:
                        eng.wait_ge(s, v)
                    ins = op.fn(eng)
                    if attach is not None:
                        ins._wait_ge(attach[1], attach[2])
                    if op.ndma:
                        if not isinstance(ins, (list, tuple)):
                            ins = [ins]
                        assert len(ins) == op.ndma, (len(ins), op.ndma)
                        for i in ins:
                            i.then_inc(dmasem[op.semkey], 16)
                    elif op.ms:
                        ins.then_inc(engsem[ename], 1)
                if ename == 'sp':
                    for k in self.used_dma:
                        eng.wait_ge(dmasem[k], pool.dma_tot[k])
                    for e2 in ENGS:
                        if e2 != 'sp' and pool.engbase[e2]:
                            eng.wait_ge(engsem[e2], pool.engbase[e2])

            @block.tensor
            def _(eng):
                run('pe', eng)

            @block.scalar
            def _(eng):
                run('act', eng)

            @block.vector
            def _(eng):
                run('dve', eng)

            @block.gpsimd
            def _(eng):
                run('pool', eng)

            @block.sync
            def _(eng):
                run('sp', eng)


class Ctx:
    pass


def _psum_banks(nc, es, n=8):
    return [es.enter_context(nc.psum_tensor('bank%d' % i, [128, 512], F32)) for i in range(n)]


def build_p2a(nc, S, C, es):
    sb = lambda name, shape, dt: es.enter_context(nc.sbuf_tensor(name, shape, dt))
    wout = sb('wout', [128, 8, D], BF16)
    g1 = sb('g1', [128, D], F32)
    b1 = sb('b1', [128, D], F32)
    wr = sb('wr', [128, 8, 20], F32)
    rb = sb('rb', [128, 20], F32)
    xt = [sb('xt%d' % i, [128, D], F32) for i in range(3)]
    rr = [sb('rr%d' % i, [128, D], F32) for i in range(3)]
    hb = [sb('hb%d' % i, [128, D], BF16) for i in range(3)]
    h32T = [sb('h32T%d' % i, [128, 8, 128], F32) for i in range(3)]
    st = [sb('st%d' % i, [128, 2, 6], F32) for i in range(3)]
    mv = [sb('mv%d' % i, [128, 2], F32) for i in range(3)]
    sm = [sb('sm%d' % i, [128, 64], F32) for i in range(3)]
    banks = C.banks

    for c in range(8):
        S.add('pool', lambda e, c=c: e.dma_start(out=wout[:, c, :], in_=C.d_wout[:, c, :]),
              writes=[('wout', c)], ndma=1, semkey=('wout', c))
    S.add('sp', lambda e: e.dma_start(out=g1[:], in_=C.d_ln[0]), writes=['g1'], ndma=1, semkey='c1')
    S.add('sp', lambda e: e.dma_start(out=b1[:], in_=C.d_ln[1]), writes=['b1'], ndma=1, semkey='c2')
    S.add('sp', lambda e: e.dma_start(out=wr[:], in_=C.d_wr), writes=['wr'], ndma=1, semkey='c3')
    S.add('sp', lambda e: e.dma_start(out=rb[:], in_=C.d_rb), writes=['rb'], ndma=1, semkey='c4')

    def tile(t):
        p = t % 3
        X, R, HB, HT, ST, MV, SM = xt[p], rr[p], hb[p], h32T[p], st[p], mv[p], sm[p]
        rX, rR, rHB, rHT, rST, rMV, rSM = ('xt', p), ('rr', p), ('hb', p), ('h32T', p), ('st', p), ('mv', p), ('sm', p)
        tok = slice(t * 128, (t + 1) * 128)
        S.add('sp', lambda e, X=X, t=t: e.dma_start(out=X[:], in_=C.d_xown[t * 128:(t + 1) * 128, :]),
              writes=[rX], ndma=1, semkey=('xt', p))
        yield
        for half in range(2):
            bk = banks[half]
            for c in range(8):
                S.add('pe', lambda e, bk=bk, c=c, half=half, tok=tok: e.matmul(
                    bk[:], lhsT=C.yT[:, c, tok], rhs=wout[:, c, half * 512:(half + 1) * 512],
                    start=(c == 0), stop=(c == 7)),
                    reads=[('wout', c), 'yT'], writes=[('bank', half)])
            S.add('dve', lambda e, bk=bk, half=half, X=X, R=R: e.scalar_tensor_tensor(
                out=R[:, half * 512:(half + 1) * 512], in0=X[:, half * 512:(half + 1) * 512], scalar=ALPHA,
                in1=bk[:], op0=ALU.mult, op1=ALU.add),
                reads=[rX, ('bank', half)], writes=[(rR, half)])
            yield
            S.add('dve', lambda e, half=half, R=R, ST=ST: e.bn_stats(out=ST[:, half, :], in_=R[:, half * 512:(half + 1) * 512]),
                  reads=[(rR, half)], writes=[(rST, half)])
            yield
        yield from _ln_tail(S, R, rR, ST, rST, MV, rMV, SM, rSM, g1, 'g1', b1, 'b1')
        S.add('act', lambda e, R=R, t=t: e.activation(out=C.ysb[:, t, :], in_=R[:], func=AF.Copy, scale=ALPHA),
              reads=[(rR, 0), (rR, 1)], writes=[('ysb', t)])
        yield
        S.add('act', lambda e, R=R, HB=HB: e.activation(out=HB[:], in_=R[:], func=AF.Copy),
              reads=[(rR, 0), (rR, 1)], writes=[rHB])
        yield
        pt = C.bank_bf[2]
        for c in range(8):
            S.add('pe', lambda e, c=c, HB=HB, pt=pt: e.transpose(pt[:, c * 128:(c + 1) * 128], HB[:, c * 128:(c + 1) * 128], C.ident_bf[:]),
                  reads=[rHB, 'ident', 'ident_f0'], writes=[('bank', 2)])
        S.add('act', lambda e, pt=pt, tok=tok: e.activation(out=C.hT[:, :, tok], in_=pt[:, 0:1024].rearrange("p (c t) -> p c t", c=8), func=AF.Copy),
              reads=[('bank', 2)], writes=[('hT', t)])
        yield
        for q4 in range(2):
            bkq = banks[3 + q4]
            for c4 in range(4):
                c = q4 * 4 + c4
                S.add('pe', lambda e, c=c, c4=c4, R=R, bkq=bkq: e.transpose(bkq[:, c4 * 128:(c4 + 1) * 128], R[:, c * 128:(c + 1) * 128], C.ident_f[:]),
                      reads=[(rR, 0), (rR, 1), 'ident', 'ident_f0'], writes=[('bank', 3 + q4)])
            S.add('dve', lambda e, bkq=bkq, q4=q4, HT=HT: e.tensor_copy(out=HT[:, q4 * 4:(q4 + 1) * 4, :], in_=bkq[:].rearrange("p (c t) -> p c t", c=4)),
                  reads=[('bank', 3 + q4)], writes=[(rHT, q4)])
            yield
        lg = banks[5]
        for c in range(8):
            S.add('pe', lambda e, c=c, HT=HT, lg=lg: e.matmul(lg[:, 0:20], lhsT=HT[:, c, :], rhs=wr[:, c, :], start=(c == 0), stop=(c == 7)),
                  reads=[(rHT, 0), (rHT, 1), 'wr'], writes=[('bank', 5)])
        yield from _router(S, C, lg, SM, rSM, rb, t)

    _rolling([(lambda t=t: tile(t)) for t in range(NT_OWN)], 3, 10)


def _ln_tail(S, R, rR, ST, rST, MV, rMV, SM, rSM, g, rg, b, rb_):
    both = [(rR, 0), (rR, 1)]
    S.add('dve', lambda e: e.bn_aggr(out=MV[:], in_=ST[:].rearrange("p a b -> p (a b)")),
          reads=[(rST, 0), (rST, 1)], writes=[rMV])
    yield
    S.add('act', lambda e: e.activation(out=SM[:, 0:1], in_=MV[:, 1:2], func=AF.Ln, bias=C_EPS_AP[0][:, 0:1], scale=1.0),
          reads=[rMV, 'eps'], writes=[(rSM, 'a')])
    yield
    S.add('act', lambda e: e.activation(out=SM[:, 1:2], in_=SM[:, 0:1], func=AF.Exp, scale=-0.5), reads=[(rSM, 'a')], writes=[(rSM, 'b')])
    yield
    S.add('dve', lambda e: e.tensor_scalar(out=R[:], in0=R[:], scalar1=MV[:, 0:1], scalar2=SM[:, 1:2], op0=ALU.subtract, op1=ALU.mult),
          reads=both + [rMV, (rSM, 'b')], writes=both)
    yield
    S.add('pool', lambda e: e.tensor_tensor(out=R[:], in0=R[:], in1=g[:], op=ALU.mult), reads=both + [rg], writes=both)
    yield
    S.add('pool', lambda e: e.tensor_tensor(out=R[:], in0=R[:], in1=b[:], op=ALU.add), reads=both + [rb_], writes=both)
    yield


C_EPS_AP = [None]


def _router(S, C, lg, SM, rSM, rb, t):
    L = SM[:, 8:28]
    r = lambda k: (rSM, k)
    S.add('dve', lambda e: e.tensor_tensor(out=L, in0=lg[:, 0:20], in1=rb[:], op=ALU.add),
          reads=[('bank', 5), 'rb'], writes=[r('L')])
    yield
    S.add('dve', lambda e: e.tensor_reduce(out=SM[:, 3:4], in_=SM[:, 8:12], axis=AX.X, op=ALU.max, negate=True), reads=[r('L')], writes=[r('nm1')])
    yield
    S.add('act', lambda e: e.activation(out=SM[:, 28:32], in_=SM[:, 8:12], func=AF.Exp, bias=SM[:, 3:4], scale=1.0, accum_out=SM[:, 4:5]),
          reads=[r('L'), r('nm1')], writes=[r('e1'), r('s1')])
    yield
    S.add('dve', lambda e: e.tensor_scalar(out=SM[:, 32:36], in0=SM[:, 8:12], scalar1=SM[:, 3:4], scalar2=0.0, op0=ALU.add, op1=ALU.is_ge), reads=[r('L'), r('nm1')], writes=[r('oh')])
    yield
    S.add('dve', lambda e: e.tensor_scalar(out=SM[:, 32:36], in0=SM[:, 32:36], scalar1=-1.0, scalar2=1e30, op0=ALU.add, op1=ALU.mult), reads=[r('oh')], writes=[r('oh')])
    yield
    S.add('dve', lambda e: e.tensor_tensor(out=SM[:, 12:28].rearrange("p (g x) -> p g x", g=4), in0=SM[:, 12:28].rearrange("p (g x) -> p g x", g=4),
                                           in1=SM[:, 32:36].unsqueeze(2).to_broadcast([128, 4, 4]), op=ALU.add),
          reads=[r('L'), r('oh')], writes=[r('L')])
    yield
    S.add('dve', lambda e: e.tensor_reduce(out=SM[:, 6:7], in_=SM[:, 12:28], axis=AX.X, op=ALU.max, negate=True), reads=[r('L')], writes=[r('nm2')])
    yield
    S.add('act', lambda e: e.activation(out=SM[:, 36:52], in_=SM[:, 12:28], func=AF.Exp, bias=SM[:, 6:7], scale=1.0),
          reads=[r('L'), r('nm2')], writes=[r('p2')])
    yield
    S.add('dve', lambda e: e.max(out=SM[:, 52:60], in_=SM[:, 36:52]), reads=[r('p2')], writes=[r('top')])
    yield
    S.add('dve', lambda e: e.scalar_tensor_tensor(out=SM[:, 7:8], in0=SM[:, 52:53], scalar=SM[:, 53:54], in1=SM[:, 4:5], op0=ALU.add, op1=ALU.mult),
          reads=[r('top'), r('s1')], writes=[r('den')])
    yield
    S.add('dve', lambda e: e.reciprocal(out=SM[:, 60:61], in_=SM[:, 7:8]), reads=[r('den')], writes=[r('wsc')])
    yield
    S.add('dve', lambda e: e.tensor_scalar(out=SM[:, 12:28], in0=SM[:, 36:52], scalar1=SM[:, 53:54], scalar2=None, op0=ALU.is_ge), reads=[r('p2'), r('top'), r('L')], writes=[r('L')])
    yield
    S.add('dve', lambda e: e.scalar_tensor_tensor(out=SM[:, 36:52], in0=SM[:, 36:52], scalar=SM[:, 60:61], in1=SM[:, 12:28], op0=ALU.mult, op1=ALU.mult),
          reads=[r('p2'), r('wsc'), r('L')], writes=[r('p2')])
    yield
    gt = C.banks[6]
    S.add('pe', lambda e: e.transpose(gt[0:16, 0:128], SM[:, 36:52], C.ident_f[:]), reads=[r('p2'), 'ident', 'ident_f0'], writes=[('bank', 6)])
    S.add('act', lambda e: e.activation(out=C.gatesT[:, t * 128:(t + 1) * 128], in_=gt[0:16, 0:128], func=AF.Copy),
          reads=[('bank', 6)], writes=[('gatesT', t)])
    yield


def build_p2b(nc, S, C, es):
    sb = lambda name, shape, dt: es.enter_context(nc.sbuf_tensor(name, shape, dt))
    wg = [sb('wg%d' % i, [128, 2, 8, DEXP], BF16) for i in range(2)]
    wu = [sb('wu%d' % i, [128, 2, 8, DEXP], BF16) for i in range(2)]
    wd = [sb('wd%d' % i, [128, 2, 2, D], BF16) for i in range(2)]
    g2 = sb('g2', [128, D], F32)
    b2 = sb('b2', [128, D], F32)
    sg = [sb('sg%d' % i, [128, 512], F32) for i in range(2)]
    t1 = [sb('t1%d' % i, [128, 512], F32) for i in range(2)]
    he = [sb('he%d' % i, [128, 256], BF16) for i in range(4)]
    st = [sb('st2%d' % i, [128, 2, 6], F32) for i in range(2)]
    mv = [sb('mv2%d' % i, [128, 2], F32) for i in range(2)]
    sm = [sb('sm2%d' % i, [128, 8], F32) for i in range(2)]
    banks = C.banks
    S.add('sp', lambda e: e.dma_start(out=g2[:], in_=C.d_ln[2]), writes=['g2'], ndma=1, semkey='c1')
    S.add('sp', lambda e: e.dma_start(out=b2[:], in_=C.d_ln[3]), writes=['b2'], ndma=1, semkey='c2')
    def ln2(t):
            p = t % 2
            ST, MV, SM = st[p], mv[p], sm[p]
            R = C.ysb[:, t, :]

            rR = ('ysbx', t)
            for half in range(2):
                S.add('dve', lambda e, half=half, R=R, ST=ST: e.bn_stats(out=ST[:, half, :], in_=R[:, half * 512:(half + 1) * 512]),
                      reads=[('ysb', t)], writes=[(('st2', p), half)])
            both = [('ysb', t)]
            S.add('dve', lambda e, MV=MV, ST=ST: e.bn_aggr(out=MV[:], in_=ST[:].rearrange("p a b -> p (a b)")),
                  reads=[(('st2', p), 0), (('st2', p), 1)], writes=[('mv2', p)])
            S.add('act', lambda e, SM=SM, MV=MV: e.activation(out=SM[:, 0:1], in_=MV[:, 1:2], func=AF.Ln, bias=C_EPS_AP[0][:, 0:1], scale=1.0),
                  reads=[('mv2', p), 'eps'], writes=[('sm2', p, 'a')])
            S.add('act', lambda e, SM=SM: e.activation(out=SM[:, 1:2], in_=SM[:, 0:1], func=AF.Exp, scale=-0.5), reads=[('sm2', p, 'a')], writes=[('sm2', p, 'b')])
            S.add('dve', lambda e, R=R, MV=MV, SM=SM: e.tensor_scalar(out=R, in0=R, scalar1=MV[:, 0:1], scalar2=SM[:, 1:2], op0=ALU.subtract, op1=ALU.mult),
                  reads=both + [('mv2', p), ('sm2', p, 'b')], writes=both)
            S.add('pool', lambda e, R=R: e.tensor_tensor(out=R, in0=R, in1=g2[:], op=ALU.mult), reads=both + ['g2'], writes=both)
            S.add('pool', lambda e, R=R: e.tensor_tensor(out=R, in0=R, in1=b2[:], op=ALU.add), reads=both + ['b2'], writes=both)
            S.add('sp', lambda e, R=R, t=t: e.dma_start(out=C.d_out[t * 128:(t + 1) * 128, :], in_=R), reads=both, writes=[('out', t)],
                  ndma=1, semkey=('out', t % 4))

    k = 0
    for ep in range(NEXP // 2):
        p = ep % 2
        S.add('pool', lambda e, p=p, ep=ep: e.dma_start(out=wg[p][:], in_=C.d_wg[2 * ep:2 * ep + 2].rearrange("e p c n -> p e c n"), max_dma_last_dim=8192),
              writes=[('wexp', p, 'g')], ndma=1, semkey=('wexp', p, 'g'))
        S.add('pool', lambda e, p=p, ep=ep: e.dma_start(out=wu[p][:], in_=C.d_wu[2 * ep:2 * ep + 2].rearrange("e p c n -> p e c n"), max_dma_last_dim=8192),
              writes=[('wexp', p, 'u')], ndma=1, semkey=('wexp', p, 'u'))
        S.add('pool', lambda e, p=p, ep=ep: e.dma_start(out=wd[p][:], in_=C.d_wd[2 * ep:2 * ep + 2].rearrange("e p c n -> p e c n"), max_dma_last_dim=8192),
              writes=[('wexp', p, 'd')], ndma=1, semkey=('wexp', p, 'd'))
        for tg in range(TOWN // 256):
            tk = slice(tg * 256, (tg + 1) * 256)
            for el in range(2):
                eg = 2 * ep + el
                gbk = 6 + (k % 2)
                for jc in range(2):
                    for c in range(8):
                        S.add('pe', lambda e, p=p, el=el, c=c, jc=jc, tk=tk: e.matmul(
                            banks[4][:, jc * 256:(jc + 1) * 256], lhsT=wg[p][:, el, c, jc * 128:(jc + 1) * 128], rhs=C.hT[:, c, tk], start=(c == 0), stop=(c == 7)),
                            reads=[('wexp', p, 'g'), ('hT', 2 * tg), ('hT', 2 * tg + 1)], writes=[('bank', 4)])
                for jc in range(2):
                    for c in range(8):
                        S.add('pe', lambda e, p=p, el=el, c=c, jc=jc, tk=tk: e.matmul(
                            banks[5][:, jc * 256:(jc + 1) * 256], lhsT=wu[p][:, el, c, jc * 128:(jc + 1) * 128], rhs=C.hT[:, c, tk], start=(c == 0), stop=(c == 7)),
                            reads=[('wexp', p, 'u'), ('hT', 2 * tg), ('hT', 2 * tg + 1)], writes=[('bank', 5)])
                S.add('pe', lambda e, eg=eg, tk=tk, gbk=gbk: e.matmul(
                    banks[gbk][:, 0:256], lhsT=C.esel[:, eg, :], rhs=C.gatesT[:, tk], start=True, stop=True),
                    reads=['esel', ('gatesT', 2 * tg), ('gatesT', 2 * tg + 1)], writes=[('bank', gbk)])
                SG, T1 = sg[k % 2], t1[k % 2]
                S.add('act', lambda e, SG=SG: e.activation(out=SG[:], in_=banks[4][:], func=AF.Silu),
                      reads=[('bank', 4)], writes=[('sg', k % 2)])
                S.add('dve', lambda e, SG=SG, T1=T1: e.tensor_tensor(out=T1[:], in0=banks[5][:], in1=SG[:], op=ALU.mult),
                      reads=[('bank', 5), ('sg', k % 2)], writes=[('t1', k % 2)])
                for jc in range(2):
                    hi = (2 * k + jc) % 4
                    HE = he[hi]
                    S.add('dve', lambda e, T1=T1, HE=HE, gbk=gbk, jc=jc: e.tensor_tensor(out=HE[:], in0=banks[gbk][:, 0:256], in1=T1[:, jc * 256:(jc + 1) * 256], op=ALU.mult),
                          reads=[('bank', gbk), ('t1', k % 2)], writes=[('he', hi)])
                for jc in range(2):
                    hi = (2 * k + jc) % 4
                    HE = he[hi]
                    first = (el == 0 and jc == 0)
                    last = (el == 1 and jc == 1)
                    for tl in range(2):
                        for half in range(2):
                            yb = tl * 2 + half
                            S.add('pe', lambda e, yb=yb, HE=HE, tl=tl, half=half, p=p, el=el, jc=jc, first=first, last=last: e.matmul(
                                banks[yb][:], lhsT=HE[:, tl * 128:(tl + 1) * 128], rhs=wd[p][:, el, jc, half * 512:(half + 1) * 512],
                                start=first, stop=last),
                                reads=[('he', hi), ('wexp', p, 'd')], writes=[('bank', yb)])
                k += 1
            for tl in range(2):
                tt = 2 * tg + tl
                for half in range(2):
                    yb = tl * 2 + half
                    S.add('dve', lambda e, yb=yb, tt=tt, half=half: e.tensor_tensor(
                        out=C.ysb[:, tt, half * 512:(half + 1) * 512], in0=banks[yb][:], in1=C.ysb[:, tt, half * 512:(half + 1) * 512], op=ALU.add),
                        reads=[('bank', yb), ('ysb', tt)], writes=[('ysb', tt)])
                if ep == NEXP // 2 - 1:
                    ln2(tt)
def consts_setup(nc, S, C, es):
    sb = lambda name, shape, dt: es.enter_context(nc.sbuf_tensor(name, shape, dt))
    C.ident_f = sb('ident_f', [128, 128], F32)
    C.ident_bf = sb('ident_bf', [128, 128], BF16)
    C.esel = sb('esel', [16, 16, 128], BF16)
    C.eps = sb('epsc', [128, 4], F32)
    C_EPS_AP[0] = C.eps
    S.add('sp', lambda e: e.dma_start(out=C.ident_f[:], in_=C.d_ident), writes=['ident_f0'], ndma=1, semkey='k1')
    S.add('pool', lambda e: e.dma_start(out=C.esel[:], in_=C.d_esel), writes=['esel'], ndma=1, semkey='k2')
    S.add('dve', lambda e: e.tensor_copy(out=C.ident_bf[:], in_=C.ident_f[:]), reads=['ident_f0'], writes=['ident'])
    S.add('dve', lambda e: e.memset(C.eps[:, 0:1], LN_EPS), writes=['eps0'])
    S.add('dve', lambda e: e.memset(C.eps[:, 1:2], 1e-6), writes=['eps1'])
    S.add('dve', lambda e: e.memset(C.eps[:, 2:3], float(np.log(128.0 ** -0.5))), writes=['eps2'])
    S.add('dve', lambda e: e.memset(C.eps[:, 3:4], 1.0), reads=['eps0', 'eps1', 'eps2'], writes=['eps'])


def host_consts():
    ident = np.eye(128, dtype=np.float32)
    esel = np.zeros((16, 16, 128), np.float32)
    for e in range(16):
        esel[e, e, :] = 1.0
    return {'c_ident': ident, 'c_esel': esel}


def declare_common_dram(nc, C):
    di = lambda name, shape: nc.dram_tensor(name, shape, F32, kind="ExternalInput").ap()
    C.d_ident = di('c_ident', [128, 128])
    C.d_esel = di('c_esel', [16, 16, 128])
    C.d_xown = di('x_own', [TOWN, D])
    C.d_wout = di('w_out', [128, 8, D])
    C.d_ln = [di('ln%d' % i, [128, D]) for i in range(4)]
    C.d_wr = di('w_r', [128, 8, 20])
    C.d_rb = di('b_r', [128, 20])
    C.d_wg = di('w_g', [NEXP, 128, 8, DEXP])
    C.d_wu = di('w_u', [NEXP, 128, 8, DEXP])
    C.d_wd = di('w_d', [NEXP, 128, 2, D])
    C.d_out = nc.dram_tensor('out', [TOWN, D], F32, kind="ExternalOutput").ap()


def host_common(inp, b, s):
    m = {}
    m.update(host_consts())
    m['x_own'] = np.ascontiguousarray(inp['x'][b, s * TOWN:(s + 1) * TOWN, :])
    m['w_out'] = np.ascontiguousarray(inp['w_out'][0].reshape(8, 128, D).transpose(1, 0, 2))
    for i, k in enumerate(['ln1_g', 'ln1_b', 'ln2_g', 'ln2_b']):
        m['ln%d' % i] = np.ascontiguousarray(np.broadcast_to(inp[k][0][None, :], (128, D)))
    wr = np.concatenate([inp['router_w1'][0]] + [inp['router_w2'][0, g] for g in range(4)], axis=1)
    m['w_r'] = np.ascontiguousarray(wr.reshape(8, 128, 20).transpose(1, 0, 2))
    br = np.concatenate([inp['router_b1'][0], inp['router_b2'][0].reshape(-1)])
    m['b_r'] = np.ascontiguousarray(np.broadcast_to(br[None, :], (128, 20)))
    m['w_g'] = np.ascontiguousarray(inp['expert_w_gate'][0].reshape(NEXP, 8, 128, DEXP).transpose(0, 2, 1, 3))
    m['w_u'] = np.ascontiguousarray(inp['expert_w_up'][0].reshape(NEXP, 8, 128, DEXP).transpose(0, 2, 1, 3))
    m['w_d'] = np.ascontiguousarray(inp['expert_w_down'][0].reshape(NEXP, 2, 128, D).transpose(0, 2, 1, 3))
    return m


def build_p2_test():
    nc = bass.Bass("TRN2", target_bir_lowering=False)
    C = Ctx()
    declare_common_dram(nc, C)
    d_yT = nc.dram_tensor('yT_dbg', [128, 8, TOWN], F32, kind="ExternalInput").ap()
    with contextlib.ExitStack() as es:
        sb = lambda name, shape, dt: es.enter_context(nc.sbuf_tensor(name, shape, dt))
        C.banks = _psum_banks(nc, es)
        C.bank_bf = [b[:].bitcast(BF16) for b in C.banks]
        C.yT = sb('yT', [128, 8, TOWN], BF16)
        P = SemPool(nc, es)
        S0 = Sched(nc, P)
        consts_setup(nc, S0, C, es)
        S0.add('pool', lambda e: e.dma_start(out=C.yT[:], in_=d_yT, max_dma_last_dim=8192), writes=['yT'], ndma=1, semkey='yT')
        S0.emit()
        nc.all_engine_barrier()
        with contextlib.ExitStack() as es2:
            sb2 = lambda name, shape, dt: es2.enter_context(nc.sbuf_tensor(name, shape, dt))
            C.ysb = sb2('ysb', [128, NT_OWN, D], F32)
            C.hT = sb2('hT', [128, 8, TOWN], BF16)
            C.gatesT = sb2('gatesT', [16, TOWN], BF16)
            with contextlib.ExitStack() as es3:
                S1 = Sched(nc, P)
                build_p2a(nc, S1, C, es3)
                S1.emit()
            nc.all_engine_barrier()
            with contextlib.ExitStack() as es4:
                S2 = Sched(nc, P)
                build_p2b(nc, S2, C, es4)
                S2.emit()
    return nc


NH = 4
HD = 128
SCALE = HD ** -0.5
NBLK = 16


def declare_p1_dram(nc, C):
    di = lambda name, shape: nc.dram_tensor(name, shape, F32, kind="ExternalInput").ap()
    C.d_xT = di('xT', [128, 8, T])
    C.d_wmb = di('w_mb', [NH, 128, 8, 384])
    C.d_cos = di('c_cos', [128, T])
    C.d_sin = di('c_sin', [128, T])
    C.d_prot = di('c_prot', [128, 128])
    C.d_cbias = di('c_cbias', [128, NBLK, NBLK])
    C.d_caus = di('c_caus', [2, 128, 256])
    C.d_sel = di('sel', [128, 2])
    C.d_xTo = di('xT_own', [128, 8, TOWN])
    C.d_coso = di('c_cos_own', [128, TOWN])
    C.d_sino = di('c_sin_own', [128, TOWN])
    C.d_cbo = di('c_cbias_own', [128, NT_OWN, NBLK])
    C.d_vmo = di('c_vmask_own', [128, 2, NT_OWN, NBLK])
    C.d_sela = di('sel_a', [128, 1])


def host_p1_consts():
    m = {}
    half = HD // 2
    inv_freq = (10000.0 ** (-np.arange(half, dtype=np.float32) / half)).astype(np.float32)
    ang = np.arange(T, dtype=np.float32)[None, :] * inv_freq[:, None]
    cos, sin = np.cos(ang).astype(np.float32), np.sin(ang).astype(np.float32)
    m['c_cos'] = np.ascontiguousarray(np.concatenate([cos, cos], 0))
    m['c_sin'] = np.ascontiguousarray(np.concatenate([-sin, sin], 0))
    prot = np.zeros((128, 128), np.float32)
    for mm in range(128):
        prot[(mm + 64) % 128, mm] = 1.0
    m['c_prot'] = prot
    cb = np.zeros((NBLK, NBLK), np.float32)
    for own in range(NBLK):
        cb[own, own:] = -1e30
    m['c_cbias'] = np.ascontiguousarray(np.broadcast_to(cb[None], (128, NBLK, NBLK)))
    caus = np.zeros((2, 128, 256), np.float32)
    for kh in range(2):
        kpos = kh * 128 + np.arange(128)[:, None]
        qpos = np.arange(256)[None, :]
        caus[kh] = np.where(kpos <= qpos, 0.0, -BIG)
    m['c_caus'] = caus
    return m


def host_p1(inp, b, s):
    m = host_p1_consts()
    m['xT'] = np.ascontiguousarray(inp['x'][b].T.reshape(8, 128, T).transpose(1, 0, 2))
    w = inp['w_in'][0]
    wm = np.stack([np.concatenate([w[:, 2056 + 128 * h:2056 + 128 * (h + 1)], w[:, 2568 + 128 * h:2568 + 128 * (h + 1)],
                                   w[:, 3080 + 128 * h:3080 + 128 * (h + 1)]], axis=1) for h in range(NH)])
    m['w_mb'] = np.ascontiguousarray(wm.reshape(NH, 8, 128, 384).transpose(0, 2, 1, 3))
    sel = np.zeros((128, 2), np.float32)
    sel[:, s] = 1.0
    m['sel'] = sel
    m['xT_own'] = np.ascontiguousarray(m['xT'][:, :, s * TOWN:(s + 1) * TOWN])
    m['c_cos_own'] = np.ascontiguousarray(m['c_cos'][:, s * TOWN:(s + 1) * TOWN])
    m['c_sin_own'] = np.ascontiguousarray(m['c_sin'][:, s * TOWN:(s + 1) * TOWN])
    cb = np.zeros((NT_OWN, NBLK), np.float32)
    vm = np.zeros((2, NT_OWN, NBLK), np.float32)
    for tt in range(NT_OWN):
        own = (NBLK // 2) * s + tt // 2
        cb[tt, own:] = -1e30
        vm[0, tt, :own] = 1.0
        vm[1, tt, own] = 1.0
    m['c_cbias_own'] = np.ascontiguousarray(np.broadcast_to(cb[None], (128, NT_OWN, NBLK)))
    m['c_vmask_own'] = np.ascontiguousarray(np.broadcast_to(vm[None], (128, 2, NT_OWN, NBLK)))
    m['sel_a'] = np.full((128, 1), 1.0 - s, np.float32)
    return m


def p1_consts_setup(nc, S, C, es):
    sb = lambda name, shape, dt: es.enter_context(nc.sbuf_tensor(name, shape, dt))
    C.xT = sb('xT_sb', [128, 8, T], BF16)
    C.sel = sb('sel_sb', [128, 2], F32)
    C.prot = sb('prot', [128, 128], BF16)
    C.caus = sb('caus', [128, 2, 256], BF16)
    C.cbias = sb('cbias', [128, NBLK, NBLK], F32)
    C.ones_bf = sb('ones_bf', [128, 128], BF16)
    S.add('sp', lambda e: e.dma_start(out=C.sel[:], in_=C.d_sel), writes=['sel'], ndma=1, semkey='k3')
    S.add('pool', lambda e: e.dma_start(out=C.prot[:], in_=C.d_prot), writes=['prot'], ndma=1, semkey='k4')
    S.add('pool', lambda e: e.dma_start(out=C.caus[:], in_=C.d_caus.rearrange("k p q -> p k q")), writes=['caus'], ndma=1, semkey='k5')
    S.add('sp', lambda e: e.dma_start(out=C.cbias[:], in_=C.d_cbias), writes=['cbias'], ndma=1, semkey='k6')
    S.add('dve', lambda e: e.memset(C.ones_bf[:], 1.0), writes=['ones'])


def build_moba(nc, S, C, es):
    sb = lambda name, shape, dt: es.enter_context(nc.sbuf_tensor(name, shape, dt))
    banks, bank_bf = C.banks, C.bank_bf
    NTL = T // 128
    HB_ = NBLK // 2
    w = [sb('wmb%d' % i, [128, 8, 384], BF16) for i in range(2)]
    xTo = sb('xTo_sb', [128, 8, TOWN], BF16)
    qT = sb('mqT', [128, TOWN], BF16)
    kT = sb('mkT', [128, T], BF16)
    vaug = sb('mv', [128, NTL, HD + 1], BF16)
    sel01 = sb('msel', [128, NT_OWN, NBLK], F32)
    cbo = sb('mcbo', [128, NT_OWN, NBLK], F32)
    vmo = sb('mvmo', [128, 2, NT_OWN, NBLK], F32)
    sela = sb('msela', [128, 1], F32)
    identA = sb('midentA', [128, 128], BF16)
    km32 = sb('km32', [128, NBLK], F32)
    kmb = sb('kmb', [128, NBLK], BF16)
    cs = [sb('cos%d' % i, [128, 512], F32) for i in range(2)]
    sn = [sb('sin%d' % i, [128, 512], F32) for i in range(2)]
    raw = [sb('raw%d' % i, [128, 512], BF16) for i in range(2)]
    ta = [sb('ta%d' % i, [128, 512], F32) for i in range(2)]
    tb = [sb('tb%d' % i, [128, 512], F32) for i in range(2)]
    gsm = [sb('gsm%d' % i, [128, 24], F32) for i in range(2)]
    PTs = [sb('PT%d' % i, [128, 512], BF16) for i in range(8)]
    Oacc = [sb('Oacc%d' % i, [128, HD + 1], F32) for i in range(8)]
    osm = [sb('mosm%d' % i, [128, 2], F32) for i in range(8)]
    ytk = [sb('mytk%d' % i, [128, HD], BF16) for i in range(8)]
    S.add('pool', lambda e: e.memset(vaug[:, :, HD:HD + 1], 1.0), writes=[('mvone',)])
    S.add('sp', lambda e: e.dma_start(out=cbo[:], in_=C.d_cbo), writes=['mcbo'], ndma=1, semkey='k11')
    S.add('sp', lambda e: e.dma_start(out=vmo[:], in_=C.d_vmo), writes=['mvmo'], ndma=1, semkey='k12')
    S.add('sp', lambda e: e.dma_start(out=sela[:], in_=C.d_sela), writes=['msela'], ndma=1, semkey='k13')
    _ts(S, 'dve', identA[:], C.ident_bf[:], sela[:, 0:1], None, ALU.mult, None, ['ident', 'msela'], ['midentA'])
    kk = [0]
    gk = 0

    def rope(W, rW, comp, dst, rdst, src, rsrc, tg, cosd, sind, cskey):
        tk = slice(tg * 512, (tg + 1) * 512)
        p = tg % 2
        S.add('sp', lambda e: [e.dma_start(out=cs[p][:], in_=cosd[:, tk]), e.dma_start(out=sn[p][:], in_=sind[:, tk])],
              writes=[('cs', p)], ndma=2, semkey=('cs', p))
        pb = kk[0] % 2
        bk = 6 + pb
        for c in range(8):
            _mm(S, banks[bk][:], W[:, c, comp * 128:(comp + 1) * 128], src[:, c, tk], c == 0, c == 7, [rW, (rsrc, tg)], [('bank', bk)])
        RAW, TA, TB = raw[pb], ta[pb], tb[pb]
        _act(S, RAW[:], banks[bk][:], AF.Copy, [('bank', bk)], [('raw', pb)])
        _mm(S, banks[bk][:], C.prot[:], RAW[:], True, True, [('raw', pb), 'prot'], [('bank', bk)])
        _tt(S, 'pool', TA[:], RAW[:], cs[p][:], ALU.mult, [('raw', pb), ('cs', p)], [('ta', pb)])
        _tt(S, 'dve', TB[:], banks[bk][:], sn[p][:], ALU.mult, [('bank', bk), ('cs', p)], [('tb', pb)])
        _tt(S, 'pool', dst[:, tk], TA[:], TB[:], ALU.add, [('ta', pb), ('tb', pb)], [(rdst, tg)])
        kk[0] += 1

    def attn(h, j, slot):
        qs = slice(j * 256, (j + 1) * 256)
        rq = ('mqT', j // 2)
        nblk = HB_ + j + 1
        for n in range(nblk):
            sbk = slot
            obk = 4 + slot
            pi = slot * 2 + (n % 2)
            Pt, rP = PTs[pi], ('PT', pi)
            for jj in range(2):
                kt = 2 * n + jj
                extra = (n == j) or (n == HB_ + j)
                _mm(S, banks[sbk][:, jj * 256:(jj + 1) * 256], kT[:, kt * 128:(kt + 1) * 128], qT[:, qs], True, not extra, [('mkT', kt // 4), rq], [('bank', sbk)])
                if n == j:
                    _mm(S, banks[sbk][:, jj * 256:(jj + 1) * 256], identA[:], C.caus[:, jj, :], False, True, ['midentA', 'caus'], [('bank', sbk)])
                elif n == HB_ + j:
                    _mm(S, banks[sbk][:, jj * 256:(jj + 1) * 256], C.ident_bf[:], C.caus[:, jj, :], False, True, ['ident', 'caus'], [('bank', sbk)])
            yield
            _act(S, Pt[:], banks[sbk][:], AF.Exp, [('bank', sbk)], [rP], scale=SCALE)
            yield
            for qt in range(2):
                for jj in range(2):
                    kt = 2 * n + jj
                    _mm(S, banks[obk][:, qt * 256:qt * 256 + HD + 1], Pt[:, jj * 256 + qt * 128:jj * 256 + (qt + 1) * 128], vaug[:, kt, :],
                        jj == 0, jj == 1, [rP, ('mv', kt // 4), ('mvone',)], [('bank', obk)])
            yield
            for qt in range(2):
                tt = 2 * j + qt
                OA, rOA = Oacc[slot * 2 + qt], ('Oacc', slot * 2 + qt)
                src = banks[obk][:, qt * 256:qt * 256 + HD + 1]
                sc_ = sel01[:, tt, n:n + 1]
                rds = [('bank', obk), ('msel', tt)]
                if n == 0:
                    _ts(S, 'dve', OA[:], src, sc_, None, ALU.mult, None, rds, [rOA])
                else:
                    _stt(S, OA[:], src, sc_, OA[:], ALU.mult, ALU.add, rds + [rOA], [rOA])
                yield
        for qt in range(2):
            tt = 2 * j + qt
            OA, rOA = Oacc[slot * 2 + qt], ('Oacc', slot * 2 + qt)
            OS, rOS = osm[slot * 2 + qt], ('mosm', slot * 2 + qt)
            YK, rYK = ytk[slot * 2 + qt], ('mytk', slot * 2 + qt)
            S.add('dve', lambda e, OS=OS, OA=OA: e.reciprocal(out=OS[:, 0:1], in_=OA[:, HD:HD + 1]), reads=[rOA], writes=[rOS])
            yield
            _ts(S, 'dve', YK[:], OA[:, 0:HD], OS[:, 0:1], None, ALU.mult, None, [rOA, rOS], [rYK])
            yield
            tbk = 4 + slot
            _tr(S, bank_bf[tbk][:, 0:128], YK[:], C.ident_bf[:], [rYK, 'ident'], [('bank', tbk)])
            _act(S, C.yT[:, 4 + h, tt * 128:(tt + 1) * 128], bank_bf[tbk][:, 0:128], AF.Copy, [('bank', tbk)], [('yT', 4 + h, tt)])
            yield

    for h in range(NH):
        W = w[h % 2]
        rW = ('wmb', h % 2)
        S.add('pool', lambda e, W=W, h=h: e.dma_start(out=W[:], in_=C.d_wmb[h], max_dma_last_dim=8192), writes=[rW], ndma=1, semkey=rW)
        if h == 0:
            for tg in range(T // 512):
                S.add('pool', lambda e, tg=tg: e.dma_start(out=C.xT[:, :, tg * 512:(tg + 1) * 512], in_=C.d_xT[:, :, tg * 512:(tg + 1) * 512]),
                      writes=[('xT', tg)], ndma=1, semkey=('xT', tg))
                if tg % 2 == 0:
                    S.add('pool', lambda e, tg=tg: e.dma_start(out=xTo[:, :, (tg // 2) * 512:(tg // 2 + 1) * 512], in_=C.d_xTo[:, :, (tg // 2) * 512:(tg // 2 + 1) * 512]),
                          writes=[('xTo', tg // 2)], ndma=1, semkey=('xTo', tg // 2))
        for tg in range(T // 512):
            rope(W, rW, 1, kT, 'mkT', C.xT, 'xT', tg, C.d_cos, C.d_sin, 0)
            if tg % 2 == 1:
                rope(W, rW, 0, qT, 'mqT', xTo, 'xTo', tg // 2, C.d_coso, C.d_sino, 1)
            for tl in range(4):
                tt = tg * 4 + tl
                for c in range(8):
                    _mm(S, banks[5][:, tl * 128:(tl + 1) * 128], C.xT[:, c, tt * 128:(tt + 1) * 128], W[:, c, 256:384], c == 0, c == 7, [rW, ('xT', tg)], [('bank', 5)])
            _act(S, vaug[:, tg * 4:(tg + 1) * 4, 0:HD], banks[5][:].rearrange("p (a d) -> p a d", a=4), AF.Copy, [('bank', 5)], [('mv', tg)])
        S.add('dve', lambda e: e.tensor_reduce(out=km32[:], in_=kT[:].rearrange("p (n k) -> p n k", k=256), axis=AX.X, op=ALU.add),
              reads=[('mkT', i) for i in range(8)], writes=['km32'])
        _act(S, kmb[:], km32[:], AF.Copy, ['km32'], ['kmb'], scale=1.0 / 256)
        for tt in range(NT_OWN):
            G = gsm[gk % 2]
            rG = ('gsm', gk % 2)
            gbk = 6 + (gk % 2)
            _mm(S, banks[gbk][:, 0:16], qT[:, tt * 128:(tt + 1) * 128], kmb[:], True, True, [('mqT', tt // 4), 'kmb'], [('bank', gbk)])
            _tt(S, 'dve', G[:, 0:16], banks[gbk][:, 0:16], cbo[:, tt, :], ALU.add, [('bank', gbk), 'mcbo'], [(rG, 'g')])
            S.add('dve', lambda e, G=G: e.max(out=G[:, 16:24], in_=G[:, 0:16]), reads=[(rG, 'g')], writes=[(rG, 't')])
            _ts(S, 'dve', G[:, 0:16], G[:, 0:16], G[:, 18:19], None, ALU.is_ge, None, [(rG, 'g'), (rG, 't')], [(rG, 'g')])
            _tt(S, 'dve', G[:, 0:16], G[:, 0:16], vmo[:, 0, tt, :], ALU.mult, [(rG, 'g'), 'mvmo'], [(rG, 'g')])
            _tt(S, 'dve', sel01[:, tt, :], G[:, 0:16], vmo[:, 1, tt, :], ALU.add, [(rG, 'g'), 'mvmo'], [('msel', tt)])
            gk += 1
        def lane(h, js, slot):
            for j in js:
                yield from attn(h, j, slot)
        _interleave([lane(h, js, i) for i, js in enumerate(((7, 0), (6, 1), (5, 2), (4, 3)))])


def build_p1_test(which='moba'):
    nc = bass.Bass("TRN2", target_bir_lowering=False)
    C = Ctx()
    declare_common_dram(nc, C)
    declare_p1_dram(nc, C)
    declare_dn_dram(nc, C)
    d_yo = nc.dram_tensor('yT_out', [128, 8, TOWN], BF16, kind="ExternalOutput").ap()
    with contextlib.ExitStack() as es:
        sb = lambda name, shape, dt: es.enter_context(nc.sbuf_tensor(name, shape, dt))
        C.banks = _psum_banks(nc, es)
        C.bank_bf = [b[:].bitcast(BF16) for b in C.banks]
        C.yT = sb('yT', [128, 8, TOWN], BF16)
        P = SemPool(nc, es)
        with contextlib.ExitStack() as es1:
            S0 = Sched(nc, P)
            consts_setup(nc, S0, C, es)
            p1_consts_setup(nc, S0, C, es1)
            S0.add('pool', lambda e: e.memset(C.yT[:], 0.0), writes=['yT'])
            if which == 'moba':
                build_moba(nc, S0, C, es1)
            if which == 'dn':
                S0.add('pool', lambda e: [e.dma_start(out=C.xT[:, c, :], in_=C.d_xT[:, c, :], max_dma_last_dim=8192) for c in range(8)],
                       writes=[('xT', c) for c in range(8)], ndma=8, semkey='xT')
                build_dn(nc, S0, C, es1)
            S0.emit()
            nc.all_engine_barrier()
            S1 = Sched(nc, P)
            S1.add('sp', lambda e: e.dma_start(out=d_yo, in_=C.yT[:]), writes=['yo'], ndma=1, semkey='yo')
            S1.emit()
    return nc


def _mm(S, out, lhsT, rhs, start, stop, reads, writes):
    return S.add('pe', lambda e: e.matmul(out, lhsT=lhsT, rhs=rhs, start=start, stop=stop), reads=reads, writes=writes)


def _tr(S, out, in_, ident, reads, writes):
    return S.add('pe', lambda e: e.transpose(out, in_, ident), reads=reads, writes=writes)


def _act(S, out, in_, func, reads, writes, bias=None, scale=1.0, accum_out=None):
    kw = {}
    if bias is not None:
        kw['bias'] = bias
    if accum_out is not None:
        kw['accum_out'] = accum_out
    return S.add('act', lambda e: e.activation(out=out, in_=in_, func=func, scale=scale, **kw), reads=reads, writes=writes)


def _tt(S, eng, out, in0, in1, op, reads, writes):
    return S.add(eng, lambda e: e.tensor_tensor(out=out, in0=in0, in1=in1, op=op), reads=reads, writes=writes)


def _ts(S, eng, out, in0, s1, s2, op0, op1, reads, writes):
    if s2 is None and eng == 'pool' and op0 == ALU.mult:
        s2, op1 = 1.0, ALU.mult
    if s2 is None:
        return S.add(eng, lambda e: e.tensor_scalar(out=out, in0=in0, scalar1=s1, scalar2=None, op0=op0), reads=reads, writes=writes)
    return S.add(eng, lambda e: e.tensor_scalar(out=out, in0=in0, scalar1=s1, scalar2=s2, op0=op0, op1=op1), reads=reads, writes=writes)


def _stt(S, out, in0, scalar, in1, op0, op1, reads, writes):
    return S.add('dve', lambda e: e.scalar_tensor_tensor(out=out, in0=in0, scalar=scalar, in1=in1, op0=op0, op1=op1), reads=reads, writes=writes)


class RR:
    def __init__(self, tiles, name):
        self.tiles = tiles
        self.name = name
        self.i = 0

    def get(self):
        k = self.i % len(self.tiles)
        self.i += 1
        return self.tiles[k], (self.name, k)


def declare_dn_dram(nc, C):
    di = lambda name, shape: nc.dram_tensor(name, shape, F32, kind="ExternalInput").ap()
    C.d_wdn = di('w_dn', [NH, 128, 8, 514])
    C.d_cw = di('c_w', [NH, 128, 3, 4])
    C.d_adt = di('a_dt', [128, 8])
    C.d_nw = di('n_w', [128, 128])
    C.d_masks = di('c_masks', [6, 128, 128])


def host_dn(inp):
    m = {}
    w = inp['w_in'][0]
    wd = np.stack([np.concatenate([w[:, 128 * h:128 * (h + 1)], w[:, 512 + 128 * h:512 + 128 * (h + 1)],
                                   w[:, 1024 + 128 * h:1024 + 128 * (h + 1)], w[:, 1536 + 128 * h:1536 + 128 * (h + 1)],
                                   w[:, 2048 + h:2049 + h], w[:, 2052 + h:2053 + h]], axis=1) for h in range(NH)])
    m['w_dn'] = np.ascontiguousarray(wd.reshape(NH, 8, 128, 514).transpose(0, 2, 1, 3))
    cw = inp['conv_w'][0]
    m['c_w'] = np.ascontiguousarray(np.stack([np.stack([cw[:, comp * 512 + h * 128: comp * 512 + (h + 1) * 128].T for comp in range(3)], axis=1)
                                              for h in range(NH)]))
    adt = np.concatenate([inp['a_log'][0], inp['dt_bias'][0]])
    m['a_dt'] = np.ascontiguousarray(np.broadcast_to(adt[None, :], (128, 8)))
    m['n_w'] = np.ascontiguousarray(np.broadcast_to(inp['dn_norm_w'][0][None, :], (128, 128)))
    idx = np.arange(128)
    same = (idx[:, None] // 64) == (idx[None, :] // 64)
    M1 = (same & (idx[:, None] <= idx[None, :])).astype(np.float32)
    M2 = (same & (idx[:, None] > idx[None, :])).astype(np.float32)
    MC0 = np.broadcast_to((idx[:, None] < 64), (128, 128)).astype(np.float32)
    MC1 = np.broadcast_to((idx[:, None] >= 64), (128, 128)).astype(np.float32)
    strict = (same & (idx[:, None] > idx[None, :])).astype(np.float32)
    incl = (same & (idx[:, None] >= idx[None, :])).astype(np.float32)
    m['c_masks'] = np.ascontiguousarray(np.stack([M1, M2, MC0, MC1, strict, incl]))
    return m


DBG = {'heads': NH, 'tiles': T // 128, 'stage': 9, 'neumann': 5}


def _interleave(gens, weights=None, offsets=None):
    gens = list(gens)
    weights = list(weights) if weights is not None else [1] * len(gens)
    offsets = list(offsets) if offsets is not None else [0] * len(gens)
    live = list(zip(gens, weights, offsets))
    rnd = 0
    while live:
        nxt = []
        for g, wgt, off in live:
            alive = True
            if rnd >= off:
                for _ in range(wgt):
                    try:
                        next(g)
                    except StopIteration:
                        alive = False
                        break
            if alive:
                nxt.append((g, wgt, off))
        live = nxt
        rnd += 1


def _rolling(thunks, width, skew):
    thunks = list(thunks)
    live = []
    rnd = 0
    last_start = -skew
    while thunks or live:
        if thunks and len(live) < width and rnd - last_start >= skew:
            live.append(thunks.pop(0)())
            last_start = rnd
        nxt = []
        for g in live:
            try:
                next(g)
                nxt.append(g)
            except StopIteration:
                pass
        live = nxt
        rnd += 1


def build_dn(nc, S, C, es):
    sb = lambda name, shape, dt: es.enter_context(nc.sbuf_tensor(name, shape, dt))
    banks, bank_bf = C.banks, C.bank_bf
    NTL = T // 128
    NS = 8
    GRP = 4
    masks = sb('dmasks', [128, 6, 128], F32)
    cw = sb('dcw', [128, NH, 3, 4], F32)
    adt = sb('dadt', [128, 8], F32)
    nA = sb('dnA', [128, 4], F32)
    nw = sb('dnw', [128, 128], F32)
    w = [sb('wdn%d' % i, [128, 8, 514], BF16) for i in range(1)] * 2
    qnT = sb('dqnT', [128, T], BF16)
    knT = sb('dknT', [128, T], BF16)
    vcT = sb('dvcT', [128, T], BF16)
    zs = sb('dzs', [128, NTL, 128], BF16)
    bg = sb('dbg', [128, NTL, 2], F32)
    sc = sb('dsc', [128, 12, NTL], F32)
    rawb = [[sb('draw%d_%d' % (c, i), [128, 515], BF16) for i in range(2)] for c in range(3)]
    dgw = sb('ddgw', [128, 3, 4, 128], BF16)
    qcs = [[sb('dqc%d_%d' % (c, i), [128, 512], BF16) for i in range(2)] for c in range(2)]
    sqs = [sb('dsq%d' % c, [128, 512], BF16) for c in range(2)]
    rns = [sb('drn%d' % c, [128, 512], F32) for c in range(2)]
    f32p = [RR([sb('df%d_%d' % (g, i), [128, 128], F32) for i in range(4)], 'df%d' % g) for g in range(GRP)]
    b16p = [RR([sb('db%d_%d' % (g, i), [128, 128], BF16) for i in range(5)], 'db%d' % g) for g in range(GRP)]
    abp = [RR([sb('dab%d_%d' % (g, i), [128, 256], BF16) for i in range(3)], 'dab%d' % g) for g in range(GRP)]
    f32s = RR([sb('dfs%d' % i, [128, 128], F32) for i in range(2)], 'dfs')
    XWs = [sb('dXW%d' % i, [128, 128], BF16) for i in range(GRP)]
    VBs = [sb('dVB%d' % i, [128, 128], BF16) for i in range(GRP)]
    DGs = [sb('dDG%d' % i, [128, 128], BF16) for i in range(GRP)]
    WT = [sb('dWT%d' % i, [128, 128], BF16) for i in range(NS)]
    BQs = [sb('dBQ%d' % i, [128, 256], BF16) for i in range(NS)]
    KD = [sb('dKD%d' % i, [128, 128], BF16) for i in range(NS)]
    QD = [sb('dQD%d' % i, [128, 128], BF16) for i in range(NS)]
    U = [sb('dU%d' % i, [128, 128], F32) for i in range(NS)]
    vnew = [sb('dvn%d' % i, [128, 128], BF16) for i in range(2)]
    otok = [sb('dot%d' % i, [128, 128], F32) for i in range(2)]
    ytok = [sb('dyt%d' % i, [128, 128], BF16) for i in range(2)]
    osm = [sb('dosm%d' % i, [128, 4], F32) for i in range(2)]
    S32 = sb('dS32', [128, 128], F32)
    Sbf = sb('dSbf', [128, 128], BF16)
    gbi = [0]
    GB = [2, 3, 4, 5, 0]

    def gbank():
        k = GB[gbi[0] % len(GB)]
        gbi[0] += 1
        return k

    S.add('sp', lambda e: e.dma_start(out=masks[:], in_=C.d_masks.rearrange("m p q -> p m q")), writes=['dmasks'], ndma=1, semkey='k7')
    S.add('sp', lambda e: e.dma_start(out=cw[:], in_=C.d_cw.rearrange("h p c k -> p h c k")), writes=['dcw'], ndma=1, semkey='k8')
    S.add('sp', lambda e: e.dma_start(out=adt[:], in_=C.d_adt), writes=['dadt'], ndma=1, semkey='k9')
    S.add('sp', lambda e: e.dma_start(out=nw[:], in_=C.d_nw), writes=['dnw'], ndma=1, semkey='k10')
    _act(S, nA[:], adt[:, 0:4], AF.Exp, ['dadt'], ['dnA0'])
    _ts(S, 'dve', nA[:], nA[:], -1.0, None, ALU.mult, None, ['dnA0'], ['dnA'])
    M1, M2, MC0, MC1, MST, MIN = (masks[:, i, :] for i in range(6))

    def prep(h, t, sl):
        f32t, b16t = f32p[t % GRP], b16p[t % GRP]
        ts_ = slice(t * 128, (t + 1) * 128)
        rq, rk, rv = ('dqnT', t // 4), ('dknT', t // 4), ('dvcT', t // 4)
        GM, rGM = f32t.get()
        _ts(S, 'pool', GM[:], M2, sc[:, 2, t:t + 1], None, ALU.mult, None, ['dmasks', 'dg'], [rGM]); yield
        b1 = gbank()
        _mm(S, banks[b1][:, 0:128], M1, GM[:], True, True, ['dmasks', rGM], [('bank', b1)])
        _mm(S, banks[b1][:, 128:256], knT[:, ts_], knT[:, ts_], True, True, [rk], [('bank', b1)])
        _mm(S, banks[b1][:, 256:384], qnT[:, ts_], knT[:, ts_], True, True, [rk, rq], [('bank', b1)]); yield
        Dm, rD = f32t.get()
        _act(S, Dm[:], banks[b1][:, 0:128], AF.Exp, [('bank', b1)], [rD]); yield
        A1, rA1 = f32t.get()
        _stt(S, A1[:], banks[b1][:, 128:256], sc[:, 0, t:t + 1], Dm[:], ALU.mult, ALU.mult, [('bank', b1), rD, 'dbeta'], [rA1]); yield
        Q1, rQ1 = f32t.get()
        _tt(S, 'dve', Q1[:], banks[b1][:, 256:384], Dm[:], ALU.mult, [('bank', b1), rD], [rQ1]); yield
        A, rA = b16t.get()
        _tt(S, 'pool', A[:], A1[:], MST, ALU.mult, [rA1, 'dmasks'], [rA]); yield
        QK, rQK = b16t.get()
        _tt(S, 'pool', QK[:], Q1[:], MIN, ALU.mult, [rQ1, 'dmasks'], [rQK]); yield
        b3 = gbank()
        _tr(S, bank_bf[b3][:, 0:128], A[:], C.ident_bf[:], [rA, 'ident'], [('bank', b3)])
        _tr(S, bank_bf[b3][:, 128:256], QK[:], C.ident_bf[:], [rQK, 'ident'], [('bank', b3)])
        _tr(S, bank_bf[b3][:, 256:384], knT[:, ts_], C.ident_bf[:], [rk, 'ident'], [('bank', b3)])
        _tr(S, bank_bf[b3][:, 384:512], vcT[:, ts_], C.ident_bf[:], [rv, 'ident'], [('bank', b3)]); yield
        BQ, rB = BQs[sl], ('dBQ', sl)
        B = BQ[:, 0:128]
        _act(S, BQ[:], bank_bf[b3][:, 0:256], AF.Copy, [('bank', b3)], [rB]); yield
        XW, rXW = XWs[t % GRP], ('dXW', t % GRP)
        _ts(S, 'dve', XW[:], bank_bf[b3][:, 256:384], sc[:, 3, t:t + 1], None, ALU.mult, None, [('bank', b3), 'dbw'], [rXW]); yield
        _act(S, KD[sl][:], bank_bf[b3][:, 256:384], AF.Copy, [('bank', b3), 'dexp'], [('dKD', sl)], scale=sc[:, 5, t:t + 1]); yield
        VB, rVB = VBs[t % GRP], ('dVB', t % GRP)
        _ts(S, 'dve', VB[:], bank_bf[b3][:, 384:512], sc[:, 0, t:t + 1], None, ALU.mult, None, [('bank', b3), 'dbeta'], [rVB]); yield
        Y, rY = b16t.get()
        _tt(S, 'pool', Y[:], C.ident_bf[:], B, ALU.subtract, ['ident', rB], [rY]); yield
        DG, rDG = DGs[t % GRP], ('dDG', t % GRP)
        _ts(S, 'pool', DG[:], C.ident_bf[:], sc[:, 4, t:t + 1], None, ALU.mult, None, ['ident', 'dexp'], [rDG]); yield
        Ak, rAk, Bk, rBk = A[:], rA, B, rB
        for k in range(DBG['neumann']):
            b4 = gbank()
            _mm(S, banks[b4][:, 0:128], Bk, Ak, True, True, [rAk, rBk], [('bank', b4)])
            if k < 4:
                _mm(S, banks[b4][:, 128:256], Ak, Bk, True, True, [rAk, rBk], [('bank', b4)])
            yield
            AB, rAB = abp[t % GRP].get()
            nc_ = 256 if k < 4 else 128
            _act(S, AB[:, 0:nc_], banks[b4][:, 0:nc_], AF.Copy, [('bank', b4)], [rAB]); yield
            A2, rA2, B2, rB2 = AB[:, 0:128], rAB, AB[:, 128:256], rAB
            b5 = gbank()
            _mm(S, banks[b5][:, 0:128], A2, Y[:], True, True, [rA2, rY], [('bank', b5)]); yield
            Y2, rY2 = b16t.get()
            _tt(S, 'dve', Y2[:], banks[b5][:, 0:128], Y[:], ALU.add, [('bank', b5), rY], [rY2]); yield
            Y, rY = Y2, rY2
            if k < 4:
                Ak, rAk, Bk, rBk = A2, rA2, B2, rB2
        TT, rTT = Y, rY
        b7 = gbank()
        _mm(S, banks[b7][:, 0:128], XW[:], TT[:], True, True, [rXW, rTT], [('bank', b7)])
        _mm(S, banks[b7][:, 128:256], TT[:], VB[:], True, True, [rVB, rTT], [('bank', b7)])
        _mm(S, banks[b7][:, 256:384], C.ones_bf[:], DG[:], True, True, [rDG, 'ones'], [('bank', b7)]); yield
        _act(S, WT[sl][:], banks[b7][:, 0:128], AF.Copy, [('bank', b7)], [('dWT', sl)]); yield
        _ts(S, 'dve', U[sl][:], banks[b7][:, 128:256], 1.0, None, ALU.mult, None, [('bank', b7)], [('dU', sl)]); yield
        _tt(S, 'dve', QD[sl][:], banks[b7][:, 256:384], qnT[:, ts_], ALU.mult, [('bank', b7), rq], [('dQD', sl)]); yield

    def scan(h, t, sl):
        pp = t % 2
        obk = 7 if pp == 0 else 1
        for c in range(2):
            rows = slice(c * 64, (c + 1) * 64)
            _mm(S, banks[6][rows, 0:128], WT[sl][:, rows], Sbf[:], True, True, [('dWT', sl), 'dSbf'], [('bank', 6)]); yield
            _tt(S, 'dve', vnew[pp][rows, :], U[sl][rows, :], banks[6][rows, 0:128], ALU.subtract, [('dU', sl), ('bank', 6)], [('dvn', pp, c)]); yield
            _mm(S, banks[6][:, 128:256], KD[sl][rows, :], vnew[pp][rows, :], True, True, [('dKD', sl), ('dvn', pp, c)], [('bank', 6)])
            _mm(S, banks[obk][rows, 0:128], QD[sl][:, rows], Sbf[:], True, False, [('dQD', sl), 'dSbf'], [('bank', obk)])
            _mm(S, banks[obk][rows, 0:128], BQs[sl][rows, 128 + c * 64:128 + (c + 1) * 64], vnew[pp][rows, :], False, True, [('dBQ', sl), ('dvn', pp, c)], [('bank', obk)]); yield
            _stt(S, Sbf[:], S32[:], sc[:, 6 + c, t:t + 1], banks[6][:, 128:256], ALU.mult, ALU.add, ['dS32', 'dexp', ('bank', 6)], ['dSbf']); yield
            _stt(S, S32[:], S32[:], sc[:, 6 + c, t:t + 1], banks[6][:, 128:256], ALU.mult, ALU.add, ['dS32', 'dexp', ('bank', 6)], ['dS32']); yield
        _act(S, otok[pp][:], banks[obk][:, 0:128], AF.Copy, [('bank', obk)], [('dot', pp)]); yield
        OS = osm[pp]
        ro = [('dot', pp)]
        J, rJ = f32s.get()
        _act(S, J[:], otok[pp][:], AF.Square, ro, [rJ, ('dosm', pp, 0)], accum_out=OS[:, 0:1]); yield
        _act(S, OS[:, 1:2], OS[:, 0:1], AF.Ln, [('dosm', pp, 0), 'eps'], [('dosm', pp, 1)], scale=1.0 / 128, bias=C.eps[:, 1:2]); yield
        _act(S, OS[:, 2:3], OS[:, 1:2], AF.Exp, [('dosm', pp, 1)], [('dosm', pp, 2)], scale=-0.5); yield
        _stt(S, ytok[pp][:], otok[pp][:], OS[:, 2:3], zs[:, t, :], ALU.mult, ALU.mult, ro + [('dosm', pp, 2), ('dzs', t)], [('dyt', pp)]); yield
        b9 = gbank()
        _tr(S, bank_bf[b9][:, 0:128], ytok[pp][:], C.ident_bf[:], [('dyt', pp), 'ident'], [('bank', b9)]); yield
        dq = slice((t % 16) * 128, (t % 16 + 1) * 128)
        rYo = ('yT', h, t % 16)
        if t < 16:
            _ts(S, 'dve', C.yT[:, h, dq], bank_bf[b9][:, 0:128], C.sel[:, 0:1], None, ALU.mult, None, [('bank', b9), 'sel'], [rYo])
        else:
            _stt(S, C.yT[:, h, dq], bank_bf[b9][:, 0:128], C.sel[:, 1:2], C.yT[:, h, dq], ALU.mult, ALU.add, [('bank', b9), 'sel', rYo], [rYo])
        yield

    def scans(h, tiles):
        for t in tiles:
            yield from scan(h, t, t % NS)

    for h in range(DBG['heads']):
        W = w[h % 2]
        rW = ('wdn', 0)
        S.add('pool', lambda e, W=W, h=h: e.dma_start(out=W[:], in_=C.d_wdn[h], max_dma_last_dim=2056), writes=[rW], ndma=1, semkey=rW)
        for comp in range(3):
            for k in range(4):
                _ts(S, 'dve' if (comp + k) % 2 else 'pool', dgw[:, comp, k, :], C.ident_bf[:], cw[:, h, comp, k:k + 1], None, ALU.mult, None,
                    ['ident', 'dcw'], [('ddgw', comp)])
            S.add('pool', lambda e, comp=comp: e.memset(rawb[comp][0][:, 0:3], 0.0), writes=[('draw', comp, 0, 'h')])
        for tg in range(T // 512):
            tk = slice(tg * 512, (tg + 1) * 512)
            p = tg % 2
            for comp in range(3):
                for c in range(8):
                    _mm(S, banks[comp][:], W[:, c, comp * 128:(comp + 1) * 128], C.xT[:, c, tk], c == 0, c == 7, [rW, ('xT', c)], [('bank', comp)])
            for comp in range(3):
                RAW = rawb[comp][p]
                _act(S, RAW[:, 3:515], banks[comp][:], AF.Copy, [('bank', comp)], [('draw', comp, p, 'm')])
                if tg < 7:
                    S.add('pool', lambda e, comp=comp, p=p, RAW=RAW: e.tensor_copy(out=rawb[comp][1 - p][:, 0:3], in_=RAW[:, 512:515]),
                          reads=[('draw', comp, p, 'm')], writes=[('draw', comp, 1 - p, 'h')])
            for comp in range(3):
                RAW = rawb[comp][p]
                for k in range(4):
                    _mm(S, banks[comp][:], dgw[:, comp, k, :], RAW[:, k:k + 512], k == 0, k == 3,
                        [('ddgw', comp), ('draw', comp, p, 'm'), ('draw', comp, p, 'h')], [('bank', comp)])
            for t in range(tg * 4, tg * 4 + 4):
                bk = 3 + (t % 2)
                for c in range(8):
                    _mm(S, banks[bk][:, 0:130], C.xT[:, c, t * 128:(t + 1) * 128], W[:, c, 384:514], c == 0, c == 7, [rW, ('xT', c)], [('bank', bk)])
                _act(S, zs[:, t, :], banks[bk][:, 0:128], AF.Silu, [('bank', bk)], [('dzs', t)])
                _ts(S, 'dve', bg[:, t, :], banks[bk][:, 128:130], 1.0, None, ALU.mult, None, [('bank', bk)], [('dbg', t)])
            _act(S, vcT[:, tk], banks[2][:], AF.Silu, [('bank', 2)], [('dvcT', tg)])
            for comp in range(2):
                _act(S, qcs[comp][p][:], banks[comp][:], AF.Silu, [('bank', comp)], [('dqc', comp, p)])
            for comp in range(2):
                _tt(S, 'pool', sqs[comp][:], qcs[comp][p][:], qcs[comp][p][:], ALU.mult, [('dqc', comp, p)], [('dsq', comp)])
                _mm(S, banks[5 + comp][:], C.ones_bf[:], sqs[comp][:], True, True, [('dsq', comp), 'ones'], [('bank', 5 + comp)])
            for comp in range(2):
                _act(S, rns[comp][:], banks[5 + comp][:], AF.Ln, [('bank', 5 + comp), 'eps'], [('drn', comp)], bias=C.eps[:, 1:2])
            for comp in range(2):
                _act(S, rns[comp][:], rns[comp][:], AF.Exp, [('drn', comp)], [('drn', comp)], scale=-0.5, bias=(C.eps[:, 2:3] if comp == 0 else None))
                dst, rdst = (qnT, 'dqnT') if comp == 0 else (knT, 'dknT')
                _tt(S, 'dve', dst[:, tk], qcs[comp][p][:], rns[comp][:], ALU.mult, [('dqc', comp, p), ('drn', comp)], [(rdst, tg)])
        if DBG['stage'] < 2:
            continue
        allbg = [('dbg', t) for t in range(NTL)]
        S.add('pool', lambda e: e.tensor_tensor(out=zs[:], in0=zs[:], in1=nw[:].unsqueeze(1).to_broadcast([128, NTL, 128]), op=ALU.mult),
              reads=[('dzs', t) for t in range(NTL)] + ['dnw'], writes=[('dzs', t) for t in range(NTL)])
        _act(S, sc[:, 1, :], bg[:, :, 0], AF.Exp, allbg, ['dsc1'], scale=-1.0)
        _ts(S, 'dve', sc[:, 1, :], sc[:, 1, :], 1.0, None, ALU.add, None, ['dsc1'], ['dsc1'])
        S.add('dve', lambda e: e.reciprocal(out=sc[:, 0, :], in_=sc[:, 1, :]), reads=['dsc1'], writes=['dbeta'])
        _act(S, sc[:, 8, :], bg[:, :, 1], AF.Exp, allbg + ['dadt'], ['dsc8'], bias=adt[:, 4 + h:5 + h])
        _act(S, sc[:, 8, :], sc[:, 8, :], AF.Ln, ['dsc8', 'eps'], ['dsc8'], bias=C.eps[:, 3:4])
        _ts(S, 'dve', sc[:, 2, :], sc[:, 8, :], nA[:, h:h + 1], None, ALU.mult, None, ['dsc8', 'dnA'], ['dg'])
        cb = 0
        for i, Mx in enumerate((M1, M2, MC0, MC1)):
            _mm(S, banks[cb][:, i * 32:(i + 1) * 32], Mx, sc[:, 2, :], True, True, ['dmasks', 'dg'], [('bank', cb)])
        _act(S, sc[:, 4:8, :], banks[cb][:, 0:128].rearrange("p (a t) -> p a t", a=4), AF.Exp, [('bank', cb)], ['dexp'])
        _tt(S, 'dve', sc[:, 3, :], sc[:, 0, :], sc[:, 4, :], ALU.mult, ['dbeta', 'dexp'], ['dbw'])
        S.add('pool', lambda e: e.memset(S32[:], 0.0), writes=['dS32'])
        S.add('pool', lambda e: e.memset(Sbf[:], 0.0), writes=['dSbf'])
        if DBG['stage'] < 3:
            continue
        ntl = DBG['tiles']
        groups = [list(range(g, min(g + GRP, ntl))) for g in range(0, ntl, GRP)]
        prev = None
        for grp in groups:
            gens = [prep(h, t, t % NS) for t in grp]
            wts = [1] * len(gens)
            if prev is not None:
                gens.append(scans(h, prev))
                wts.append(2)
            _interleave(gens, wts)
            prev = grp
        _interleave([scans(h, prev)])


def build_full():
    nc = bass.Bass("TRN2", target_bir_lowering=False)
    C = Ctx()
    declare_common_dram(nc, C)
    declare_p1_dram(nc, C)
    declare_dn_dram(nc, C)
    with contextlib.ExitStack() as es:
        sb = lambda name, shape, dt: es.enter_context(nc.sbuf_tensor(name, shape, dt))
        C.banks = _psum_banks(nc, es)
        C.bank_bf = [b[:].bitcast(BF16) for b in C.banks]
        C.yT = sb('yT', [128, 8, TOWN], BF16)
        P = SemPool(nc, es)
        with contextlib.ExitStack() as es1:
            with contextlib.ExitStack() as esm:
                Sm = Sched(nc, P)
                consts_setup(nc, Sm, C, es)
                p1_consts_setup(nc, Sm, C, es1)
                build_moba(nc, Sm, C, esm)
                Sm.add('pool', lambda e: e.memset(C.yT[:, 0:4, :], 0.0), writes=[('yT', c, i) for c in range(4) for i in range(16)])
                Sm.emit()
            nc.all_engine_barrier()
            with contextlib.ExitStack() as esd:
                Sd = Sched(nc, P)
                build_dn(nc, Sd, C, esd)
                Sd.emit()
            nc.all_engine_barrier()
        with contextlib.ExitStack() as es2:
            sb2 = lambda name, shape, dt: es2.enter_context(nc.sbuf_tensor(name, shape, dt))
            C.ysb = sb2('ysb', [128, NT_OWN, D], F32)
            C.hT = sb2('hT', [128, 8, TOWN], BF16)
            C.gatesT = sb2('gatesT', [16, TOWN], BF16)
            with contextlib.ExitStack() as es3:
                S1 = Sched(nc, P)
                build_p2a(nc, S1, C, es3)
                S1.emit()
            nc.all_engine_barrier()
            with contextlib.ExitStack() as es4:
                S2 = Sched(nc, P)
                build_p2b(nc, S2, C, es4)
                S2.emit()
    return nc


_NC_CACHE = [None]


def kernel(**inputs):
    inp = {k: np.asarray(v) for k, v in inputs.items()}
    if _NC_CACHE[0] is None:
        _NC_CACHE[0] = build_full()
    nc = _NC_CACHE[0]
    dn = host_dn(inp)
    maps = []
    for c in range(8):
        b, s = c // 2, c % 2
        m = host_common(inp, b, s)
        m.update(host_p1(inp, b, s))
        m.update(dn)
        maps.append(m)
    res = run_bass_kernel_spmd(nc, maps, core_ids=list(range(8)))
    out = np.empty((4, T, D), np.float32)
    for c in range(8):
        b, s = c // 2, c % 2
        out[b, s * TOWN:(s + 1) * TOWN, :] = np.asarray(res.results[c]['out'])
    return out
```
